# Optimizing a Trainium2 kernel written in Bass

```python
import math
import jax, jax.numpy as jnp
from jax import lax
import numpy as np

D_MODEL = 1024
BATCH = 32
SEQ = 2048
DEPTH = 1

D_MIX = 2 * D_MODEL
SSD_WIDTH = D_MIX // 2
SSD_HEAD_DIM = 64
SSD_HEADS = SSD_WIDTH // SSD_HEAD_DIM
SSD_GROUPS = 2
SSD_STATE = 128
CONV_WIDTH = 4
CONV_DIM = SSD_WIDTH + 2 * SSD_GROUPS * SSD_STATE
RET_WIDTH = D_MIX - SSD_WIDTH
RET_HEADS = 4
RET_HEAD_DIM = RET_WIDTH // RET_HEADS
CHUNK = 128
IN_COLS = SSD_WIDTH + CONV_DIM + SSD_HEADS + 4 * RET_WIDTH
N_MEM = 256
XATTN_HEADS = 4
XATTN_HEAD_DIM = D_MODEL // XATTN_HEADS
D_FF = ((8 * D_MODEL // 3 + 255) // 256) * 256
ROPE_BASE = 10000.0
EPS = 1e-6
POS_OFFSET_MAX = 1024

kernel_name = "hybrid_ssd_retention_xattn_block"


def _rms(x, eps=EPS):
    xf = x.astype(jnp.float32)
    return xf * lax.rsqrt(jnp.mean(xf * xf, axis=-1, keepdims=True) + eps)


def _rmsnorm(x, g):
    return (_rms(x) * g.astype(jnp.float32)).astype(x.dtype)


def _causal_dwconv(u, w, b):
    c = u.shape[-1]
    y = lax.conv_general_dilated(
        u, w[:, None, :].astype(u.dtype), window_strides=(1,),
        padding=[(CONV_WIDTH - 1, 0)], dimension_numbers=("NWC", "WIO", "NWC"),
        feature_group_count=c)
    return y + b.astype(u.dtype)


def _ssd_scan(x_dt, da, bmat, cmat):
    bsz, L, H, P = x_dt.shape
    nc = L // CHUNK
    r = H // SSD_GROUPS
    to_chunks = lambda t, shp: jnp.moveaxis(t.reshape((bsz, nc, CHUNK) + shp), 1, 0)
    xc_all = to_chunks(x_dt, (SSD_GROUPS, r, P))
    da_all = to_chunks(da, (SSD_GROUPS, r))
    b_all = to_chunks(bmat, (SSD_GROUPS, SSD_STATE))
    c_all = to_chunks(cmat, (SSD_GROUPS, SSD_STATE))
    causal = jnp.tril(jnp.ones((CHUNK, CHUNK), dtype=bool))[None, :, :, None, None]

    def step(state, inp):
        xc, dac, bc, cc = inp
        acs = jnp.cumsum(dac, axis=1)
        seg = acs[:, :, None] - acs[:, None, :]
        decay = jnp.exp(jnp.where(causal, seg, -jnp.inf))
        cb = jnp.einsum("btgn,bsgn->btsg", cc, bc)
        y_diag = jnp.einsum("btsg,btsgr,bsgrp->btgrp", cb, decay, xc)
        y_off = jnp.einsum("btgn,bgrpn,btgr->btgrp", cc, state, jnp.exp(acs))
        last = acs[:, -1]
        w_s = jnp.exp(last[:, None] - acs)
        new_state = state * jnp.exp(last)[..., None, None] + jnp.einsum(
            "bsgn,bsgr,bsgrp->bgrpn", bc, w_s, xc)
        return new_state, y_diag + y_off

    state0 = jnp.zeros((bsz, SSD_GROUPS, r, P, SSD_STATE), jnp.float32)
    _, y = lax.scan(step, state0, (xc_all, da_all, b_all, c_all))
    return jnp.moveaxis(y, 0, 1).reshape(bsz, L, H, P)


def _rotate(t, pos):
    half = t.shape[-1] // 2
    inv_freq = 1.0 / (ROPE_BASE ** jnp.linspace(0.0, 1.0, half, dtype=jnp.float32))
    ang = pos[:, :, None, None] * inv_freq
    cos, sin = jnp.cos(ang), jnp.sin(ang)
    t0, t1 = t[..., 0::2], t[..., 1::2]
    return jnp.stack([t0 * cos - t1 * sin, t1 * cos + t0 * sin], axis=-1).reshape(t.shape)


def _retention_scan(q, k, v):
    bsz, L, H, dk = q.shape
    dv = v.shape[-1]
    nc = L // CHUNK
    log_gamma = jnp.log(1.0 - 2.0 ** (-5.0 - jnp.arange(H, dtype=jnp.float32)))
    idx = jnp.arange(CHUNK, dtype=jnp.float32)
    rel = idx[:, None] - idx[None, :]
    intra = jnp.where(rel[None] >= 0,
                      jnp.exp(jnp.maximum(rel, 0.0)[None] * log_gamma[:, None, None]), 0.0)
    xi = jnp.exp((idx + 1.0)[None] * log_gamma[:, None])
    zeta = jnp.exp((CHUNK - 1.0 - idx)[None] * log_gamma[:, None])
    chunk_decay = jnp.exp(CHUNK * log_gamma)
    to_chunks = lambda t: t.reshape(bsz, nc, CHUNK, H, -1).transpose(1, 0, 3, 2, 4)

    def step(state, inp):
        qc, kc, vc = inp
        scores = jnp.einsum("bhtd,bhsd->bhts", qc, kc) * intra
        out = jnp.einsum("bhts,bhse->bhte", scores, vc) + \
            jnp.einsum("bhtd,bhde->bhte", qc, state) * xi[..., None]
        new_state = state * chunk_decay[:, None, None] + \
            jnp.einsum("bhsd,hs,bhse->bhde", kc, zeta, vc)
        return new_state, out

    state0 = jnp.zeros((bsz, H, dk, dv), jnp.float32)
    _, o = lax.scan(step, state0, (to_chunks(q), to_chunks(k), to_chunks(v)))
    return o.transpose(1, 0, 3, 2, 4).reshape(bsz, L, H, dv)


def _hybrid_mixer(hn, pos, w_in, conv_w, conv_b, dt_bias, a_log, d_skip, ssd_norm_g, w_out):
    bsz, L, _ = hn.shape
    proj = jnp.einsum("bld,de->ble", hn, w_in)
    o0 = SSD_WIDTH
    o1 = o0 + CONV_DIM
    o2 = o1 + SSD_HEADS
    z = proj[..., :o0]
    xbc = proj[..., o0:o1]
    dt_raw = proj[..., o1:o2]
    q, k, v, g = jnp.split(proj[..., o2:], 4, axis=-1)

    xbc = jax.nn.silu(_causal_dwconv(xbc, conv_w, conv_b)).astype(jnp.float32)
    xs = xbc[..., :SSD_WIDTH].reshape(bsz, L, SSD_HEADS, SSD_HEAD_DIM)
    bmat = xbc[..., SSD_WIDTH:SSD_WIDTH + SSD_GROUPS * SSD_STATE].reshape(bsz, L, SSD_GROUPS, SSD_STATE)
    cmat = xbc[..., SSD_WIDTH + SSD_GROUPS * SSD_STATE:].reshape(bsz, L, SSD_GROUPS, SSD_STATE)
    dt = jax.nn.softplus(dt_raw.astype(jnp.float32) + dt_bias.astype(jnp.float32))
    a = -jnp.exp(a_log.astype(jnp.float32))
    y = _ssd_scan(xs * dt[..., None], dt * a, bmat, cmat)
    y = y + d_skip.astype(jnp.float32)[:, None] * xs
    y = y.reshape(bsz, L, SSD_WIDTH) * jax.nn.silu(z.astype(jnp.float32))
    y_ssd = (_rms(y) * ssd_norm_g.astype(jnp.float32)).astype(hn.dtype)

    qr = _rotate(q.astype(jnp.float32).reshape(bsz, L, RET_HEADS, RET_HEAD_DIM), pos)
    kr = _rotate(k.astype(jnp.float32).reshape(bsz, L, RET_HEADS, RET_HEAD_DIM), pos) * (RET_HEAD_DIM ** -0.5)
    vr = v.astype(jnp.float32).reshape(bsz, L, RET_HEADS, RET_HEAD_DIM)
    o = _retention_scan(qr, kr, vr)
    o = _rms(o).reshape(bsz, L, RET_WIDTH)
    y_ret = (jax.nn.silu(g.astype(jnp.float32)) * o).astype(hn.dtype)

    return jnp.einsum("ble,ed->bld", jnp.concatenate([y_ssd, y_ret], axis=-1), w_out)


def _cross_attn(hn, mem, wq, wk, wv, wo):
    bsz, L, _ = hn.shape
    q = (hn @ wq).reshape(bsz, L, XATTN_HEADS, XATTN_HEAD_DIM)
    k = (mem @ wk).reshape(bsz, -1, XATTN_HEADS, XATTN_HEAD_DIM)
    v = (mem @ wv).reshape(bsz, -1, XATTN_HEADS, XATTN_HEAD_DIM)
    s = jnp.einsum("blhd,bmhd->bhlm", q, k).astype(jnp.float32) * (XATTN_HEAD_DIM ** -0.5)
    p = jax.nn.softmax(s, axis=-1).astype(v.dtype)
    o = jnp.einsum("bhlm,bmhd->blhd", p, v).reshape(bsz, L, D_MODEL)
    return o @ wo


def _swiglu(hn, w_gate, w_up, w_down):
    return (jax.nn.silu(hn @ w_gate) * (hn @ w_up)) @ w_down


def setup_inputs(seed: int = 0) -> dict:
    key = jax.random.key(seed)
    ks = jax.random.split(key, 24)
    nrm = lambda k, shp, fan: jax.random.normal(k, shp, jnp.float32) * (fan ** -0.5)
    gain = lambda k, shp: 1.0 + 0.05 * jax.random.normal(k, shp, jnp.float32)
    x = jax.random.normal(ks[0], (BATCH, SEQ, D_MODEL), jnp.float32)
    mem = jax.random.normal(ks[1], (BATCH, N_MEM, D_MODEL), jnp.float32)
    offset = jax.random.randint(ks[2], (BATCH, 1), 0, POS_OFFSET_MAX, dtype=jnp.int32)
    positions = offset + jnp.arange(SEQ, dtype=jnp.int32)[None, :]
    dt0 = jnp.exp(jax.random.uniform(ks[6], (DEPTH, SSD_HEADS), jnp.float32,
                                     math.log(1e-3), math.log(1e-1)))
    dt_bias = dt0 + jnp.log(-jnp.expm1(-dt0))
    a_log = jnp.log(jax.random.uniform(ks[7], (DEPTH, SSD_HEADS), jnp.float32, 1.0, 16.0))
    return {
        "x": x,
        "mem": mem,
        "positions": positions,
        "norm_mix_g": gain(ks[3], (DEPTH, D_MODEL)),
        "w_in": nrm(ks[4], (DEPTH, D_MODEL, IN_COLS), D_MODEL),
        "conv_w": nrm(ks[5], (DEPTH, CONV_WIDTH, CONV_DIM), CONV_WIDTH),
        "conv_b": 0.02 * jax.random.normal(ks[8], (DEPTH, CONV_DIM), jnp.float32),
        "dt_bias": dt_bias,
        "a_log": a_log,
        "d_skip": gain(ks[9], (DEPTH, SSD_HEADS)),
        "ssd_norm_g": gain(ks[10], (DEPTH, SSD_WIDTH)),
        "w_out": nrm(ks[11], (DEPTH, D_MIX, D_MODEL), D_MIX),
        "norm_xattn_g": gain(ks[12], (DEPTH, D_MODEL)),
        "w_xq": nrm(ks[13], (DEPTH, D_MODEL, D_MODEL), D_MODEL),
        "w_xk": nrm(ks[14], (DEPTH, D_MODEL, D_MODEL), D_MODEL),
        "w_xv": nrm(ks[15], (DEPTH, D_MODEL, D_MODEL), D_MODEL),
        "w_xo": nrm(ks[16], (DEPTH, D_MODEL, D_MODEL), D_MODEL),
        "norm_ffn_g": gain(ks[17], (DEPTH, D_MODEL)),
        "w_gate": nrm(ks[18], (DEPTH, D_MODEL, D_FF), D_MODEL),
        "w_up": nrm(ks[19], (DEPTH, D_MODEL, D_FF), D_MODEL),
        "w_down": nrm(ks[20], (DEPTH, D_FF, D_MODEL), D_FF),
        "norm_final_g": gain(ks[21], (D_MODEL,)),
    }


def reference(x, mem, positions, norm_mix_g, w_in, conv_w, conv_b, dt_bias, a_log, d_skip,
              ssd_norm_g, w_out, norm_xattn_g, w_xq, w_xk, w_xv, w_xo, norm_ffn_g,
              w_gate, w_up, w_down, norm_final_g):
    pos = positions.astype(jnp.float32)
    h = x
    for i in range(DEPTH):
        hn = _rmsnorm(h, norm_mix_g[i])
        h = h + _hybrid_mixer(hn, pos, w_in[i], conv_w[i], conv_b[i], dt_bias[i], a_log[i],
                              d_skip[i], ssd_norm_g[i], w_out[i])
        hn = _rmsnorm(h, norm_xattn_g[i])
        h = h + _cross_attn(hn, mem, w_xq[i], w_xk[i], w_xv[i], w_xo[i])
        hn = _rmsnorm(h, norm_ffn_g[i])
        h = h + _swiglu(hn, w_gate[i], w_up[i], w_down[i])
    return _rmsnorm(h, norm_final_g)
```

```python
import math
import numpy as np
from contextlib import ExitStack

import concourse.bass as bass
import concourse.mybir as mybir
from concourse.bass_utils import run_bass_kernel_spmd

F32 = mybir.dt.float32
BF16 = mybir.dt.bfloat16
I32 = mybir.dt.int32
AF = mybir.ActivationFunctionType
ALU = mybir.AluOpType

NCORES = 8
D = 1024
SEQ = 2048
BATCH = 32
NMEM = 256
DFF = 2816
T = 512
CH = 4
EPS = 1e-6
EPOCH = 12000
NSLOT = 4
SLOT_ELEMS = 4096
TWO_PI = 2.0 * math.pi


class Prog:
    ENG = ("pe", "act", "dve", "pool", "sp")

    def __init__(self):
        self.streams = {e: [] for e in self.ENG}
        self.cnt = {}
        self.state = {}
        self.waited = {e: {} for e in self.ENG}
        self.dma_streams = []
        self.final = []
        self.unordered = set()

    def _conf(self, key):
        page, ns, sub = key
        d = self.state.setdefault(page, {})
        out = []
        for (ns2, sub2), st in d.items():
            if ns2 != ns or sub is None or sub2 is None or sub2 == sub:
                out.append(st)
        return out

    def _get(self, key):
        page, ns, sub = key
        d = self.state.setdefault(page, {})
        if (ns, sub) not in d:
            d[(ns, sub)] = [None, {}, {}]
        return d[(ns, sub)]

    @staticmethod
    def _norm(k):
        if isinstance(k, str):
            return (k, "", None)
        if len(k) == 2:
            return (k[0], "", k[1])
        return k

    def op(self, eng, fn, reads=(), writes=(), dma=None):
        reads = [self._norm(k) for k in reads]
        writes = [self._norm(k) for k in writes]
        xreads = [k for k in reads if k[0].startswith("ps")]
        reads = [k for k in reads if not k[0].startswith("ps")]
        veng = dma if dma is not None else eng
        deps = {}

        def add(dep):
            if dep is None:
                return
            v, i = dep
            if deps.get(v, 0) < i:
                deps[v] = i

        for r in reads:
            for st in self._conf(r):
                add(st[0])
        for x in xreads:
            for st in self._conf(x):
                add(st[0])
                for v, i in st[2].items():
                    if v != veng:
                        add((v, i))
        for w in writes:
            for st in self._conf(w):
                add(st[0])
                for v, i in st[1].items():
                    add((v, i))
                for v, i in st[2].items():
                    add((v, i))
        waits = []
        for v, i in deps.items():
            if v == "pe" and eng == "pe":
                continue
            if self.waited[eng].get(v, 0) >= i:
                continue
            self.waited[eng][v] = i
            waits.append((v, i))
        if dma is not None and dma not in self.cnt:
            self.dma_streams.append(dma)
        self.cnt[veng] = self.cnt.get(veng, 0) + 1
        idx = self.cnt[veng]
        for r in reads:
            st = self._get(r)
            if st[1].get(veng, 0) < idx:
                st[1][veng] = idx
        for x in xreads:
            st = self._get(x)
            if st[2].get(veng, 0) < idx:
                st[2][veng] = idx
        for w in writes:
            page, ns, sub = w
            if sub is None:
                d = self.state.setdefault(page, {})
                for k2 in [k2 for k2 in d if k2[0] == ns and k2[1] is not None]:
                    del d[k2]
            st = self._get(w)
            st[0] = (veng, idx)
            st[1] = {}
            st[2] = {}
        self.streams[eng].append((waits, fn, veng, idx))

    def emit(self, nc, es):
        sems = {}
        for e in ("pe", "act", "dve", "pool"):
            n = self.cnt.get(e, 0)
            ne = max(1, (n + EPOCH - 1) // EPOCH)
            sems[e] = [es.enter_context(nc.semaphore(f"s_{e}{k}")) for k in range(ne)]
        for d in self.dma_streams:
            sems[d] = [es.enter_context(nc.semaphore(f"d_{d}"))]

        def semval(v, i):
            if v in ("pe", "act", "dve", "pool"):
                return sems[v][(i - 1) // EPOCH], (i - 1) % EPOCH + 1
            if v in self.unordered:
                return sems[v][0], 16 * self.cnt[v]
            return sems[v][0], 16 * i

        block = es.enter_context(nc.Block())

        def runner(name):
            def f(e):
                for (waits, fn, veng, idx) in self.streams[name]:
                    for (v, i) in waits:
                        s, val = semval(v, i)
                        e.wait_ge(s, val)
                    ins = fn(e)
                    s, val = semval(veng, idx)
                    if veng in ("pe", "act", "dve", "pool"):
                        ins.then_inc(s, 1)
                    else:
                        ins.then_inc(s, 16)
                for (fe, d) in self.final:
                    if fe == name:
                        s, val = semval(d, self.cnt[d])
                        e.wait_ge(s, val)
            return f

        block.tensor(runner("pe"))
        block.scalar(runner("act"))
        block.vector(runner("dve"))
        block.gpsimd(runner("pool"))
        block.sync(runner("sp"))


def bc(ap, axis, shape):
    return ap.unsqueeze(axis).to_broadcast(shape)


def build_program(nseq=4, nblk=4, do_cast=True):
    nc = bass.Bass("TRN2", target_bir_lowering=False)
    P = Prog()
    es = ExitStack()
    L = nblk * T

    def din(name, shape, dt=F32):
        return nc.dram_tensor(name, list(shape), dt, kind="ExternalInput").ap()

    x_d = din("x", [nseq, L, D])
    mem_d = din("mem", [nseq, NMEM, D])
    pos_d = din("pos", [nseq, L], I32)
    out_d = nc.dram_tensor("out", [nseq, L, D], F32, kind="ExternalOutput").ap()
    WSPEC = {
        "w_fm": (1024, 3584), "w_tm": (1024, 3072), "w_out": (2048, 1024), "w_xq": (1024, 1024),
        "w_xk": (1024, 1024), "w_xv": (1024, 1024), "w_xo": (1024, 1024), "w_gu": (1024, 5632),
        "w_down": (2816, 1024), "w_dt": (1024, 16),
    }
    wf = {k: din(k, v) for k, v in WSPEC.items()}
    wb = {k: nc.dram_tensor(k + "_b", list(v), BF16, kind="Internal").ap() for k, v in WSPEC.items()}
    c_ident = din("c_ident", [128, 128])
    c_U = din("c_U", [128, 128])
    c_MGT = din("c_MGT", [128, 128])
    c_intraT = din("c_intraT", [128, 4, 128])
    c_xi = din("c_xi", [128, 4, 128])
    c_zeta = din("c_zeta", [128, 4])
    c_invf = din("c_invf", [128, 1])
    g_mix = din("g_mix", [128, 8])
    g_xat = din("g_xat", [128, 8])
    g_ffn = din("g_ffn", [128, 8])
    g_ssd = din("g_ssd", [128, 8])
    g_fin = din("g_fin", [128, 1024])
    convw_d = din("convw", [128, 12, 4])
    convb_d = din("convb", [128, 12])
    dtb_d = din("dtb", [128, 16])
    alog_d = din("alog", [128, 16])
    dsk_d = din("dsk", [128, 16])

    def sb(name, shape, dt=F32):
        return es.enter_context(nc.sbuf_tensor(name, list(shape), dt))

    ident = sb("ident", [128, 128], BF16)
    U = sb("U", [128, 128])
    MGT = sb("MGT", [128, 128])
    onesf = sb("onesf", [128, 128])
    onesb = sb("onesb", [128, 128], BF16)
    intraT = sb("intraT", [128, 4, 128])
    xi_b = sb("xi_b", [128, 4, 128])
    zeta16 = sb("zeta16", [128, 4])
    invf = sb("invf", [128, 1])
    gmix = sb("gmix", [128, 8])
    gxat = sb("gxat", [128, 8])
    gffn = sb("gffn", [128, 8])
    gssd = sb("gssd", [128, 8])
    gfin = sb("gfin", [128, 1024])
    convw = sb("convw_s", [128, 12, 4])
    convb = sb("convb_s", [128, 12])
    dtb = sb("dtb_s", [128, 16])
    a_b = sb("a_b", [128, 16])
    dsk = sb("dsk_s", [128, 16])
    neghalf = sb("neghalf", [128, 4])
    negone = sb("negone", [128, CH * 16])
    wdt = sb("wdt", [128, 8, 16], BF16)
    wring = [sb(f"wring{i}", [128, SLOT_ELEMS], BF16) for i in range(NSLOT)]
    h = sb("h", [128, CH, D])
    hs = sb("hs", [128, D], BF16)
    hnT = sb("hnT", [128, 8, T], BF16)
    ubuf = [sb(f"ubuf{i}", [128, T + 3]) for i in range(2)]
    hist = sb("hist", [128, 12, 3])
    RA = sb("RA", [128, 4, 4096], BF16)
    RB = sb("RB", [128, 4, 4096], BF16)
    RC = sb("RC", [128, 4, 512])
    Bfm = sb("Bfm", [128, 2, T], BF16)
    Cfm = sb("Cfm", [128, 2, T], BF16)
    cosT = sb("cosT", [128, T])
    sinT = sb("sinT", [128, T])
    xs_tm = sb("xs_tm", [128, D], BF16)
    xdt = sb("xdt", [128, D], BF16)
    kz_tm = sb("kz_tm", [128, D], BF16)
    B_tm = sb("B_tm", [128, 256], BF16)
    qx = sb("qx", [128, 8, 128], BF16)
    eseg = sb("eseg", [128, 16, 128], BF16)
    CBm = sb("CBm", [128, 2, 128], BF16)
    xw = sb("xw", [128, D], BF16)
    t12 = sb("t12", [128, 2, D])
    Ss = sb("Ss", [128, D])
    Ssb = sb("Ssb", [128, D], BF16)
    Sr = sb("Sr", [128, 4, 512])
    Srb = sb("Srb", [128, 4, 512], BF16)
    scm = sb("scm", [128, 4, 128], BF16)
    yr = sb("yr", [128, D], BF16)
    KT = sb("KT", [128, 8, NMEM], BF16)
    Vm = sb("Vm", [128, 2, D], BF16)
    dtt = sb("dtt", [128, CH, 16])
    da = sb("da", [128, CH, 16])
    spt = [sb(f"spt{i}", [128, CH * 16]) for i in range(4)]
    e3 = sb("e3", [128, 48])
    rs_ssd = sb("rs_ssd", [128, CH])
    smalls = [sb(f"sm{i}", [128, 4]) for i in range(8)]
    sm_ctr = [0]

    def small():
        sm_ctr[0] += 1
        i = sm_ctr[0] % len(smalls)
        return smalls[i], f"sm{i}"

    q_rot = RA[:, 0, :].rearrange("p (a t) -> p a t", a=8)
    k_rot = RA[:, 1, :].rearrange("p (a t) -> p a t", a=8)
    yT = RA[:, 2:4, :].rearrange("p c (a t) -> p (c a) t", a=8)
    RAflat = RA[:, :, :].rearrange("p c e -> p (c e)")
    act = RAflat[:, 0:22 * T].rearrange("p (a t) -> p a t", a=22)
    zs = RB[:, 0, :].rearrange("p (c f) -> p c f", c=CH)
    vt = RB[:, 1, :].rearrange("p (c f) -> p c f", c=CH)
    gs = RB[:, 2, :].rearrange("p (c f) -> p c f", c=CH)
    xs_fm = RB[:, 3, :].rearrange("p (a t) -> p a t", a=8)
    QT = RB[:, 0, :].rearrange("p (a t) -> p a t", a=8)
    PT = RB[:, 1, :].rearrange("p (a t) -> p a t", a=8)
    rinv = RB[:, 2, :].bitcast(F32).rearrange("p (a t) -> p a t", a=4)
    oTn = RB[:, 3, :].rearrange("p (a t) -> p a t", a=8)
    ubuf3 = eseg[:, :, :].rearrange("p a t -> p (a t)").bitcast(F32)[:, 0:T + 3]
    eseg2 = t12[:, 1, :].bitcast(BF16).rearrange("p (a t) -> p a t", a=16)
    xpre = RB[:, 0:2, :].bitcast(F32).rearrange("p a (c f) -> p (a c) f", f=D)
    rhs_cum = RC[:, :, :].rearrange("p a (b t) -> p (a b) t", b=4)
    memT = RC[:, :, :].bitcast(BF16)[:, 0:2, :].rearrange("p a (k m) -> p (a k) m", m=NMEM)
    memst = t12
    ysc = hs
    sg = hs[:, 0:T]
    identf = t12[:, 0, 0:128]

    PS = [es.enter_context(nc.psum_tensor(f"ps{i}", [128, 512], F32)) for i in range(8)]
    free_banks = list(range(8))

    def bank():
        assert free_banks, "PSUM exhausted"
        i = free_banks.pop(0)
        return PS[i], f"ps{i}"

    def rel(*pks):
        for pk in pks:
            i = int(pk[2:])
            assert i not in free_banks
            free_banks.append(i)

    def psb(p):
        return p[:].bitcast(BF16)

    def dma_in(stream, out_ap, in_ap, writes, reads=()):
        P.op("sp", lambda e: e.dma_start(out=out_ap, in_=in_ap), reads=reads, writes=writes, dma=stream)

    def act_op(out, in_, func, reads, writes, **kw):
        P.op("act", lambda e: e.activation(out=out, in_=in_, func=func, **kw), reads=reads, writes=writes)

    def tt(eng, out, in0, in1, op, reads, writes):
        P.op(eng, lambda e: e.tensor_tensor(out=out, in0=in0, in1=in1, op=op), reads=reads, writes=writes)

    def ts(eng, out, in0, s1, s2, op0, op1, reads, writes):
        if s2 is None:
            P.op(eng, lambda e: e.tensor_scalar(out=out, in0=in0, scalar1=s1, scalar2=None, op0=op0),
                 reads=reads, writes=writes)
        else:
            P.op(eng, lambda e: e.tensor_scalar(out=out, in0=in0, scalar1=s1, scalar2=s2, op0=op0, op1=op1),
                 reads=reads, writes=writes)

    def stt(out, in0, scalar, in1, op0, op1, reads, writes):
        P.op("dve", lambda e: e.scalar_tensor_tensor(out=out, in0=in0, scalar=scalar, in1=in1, op0=op0, op1=op1),
             reads=reads, writes=writes)

    def copy(eng, out, in_, reads, writes):
        if eng == "act":
            act_op(out, in_, AF.Copy, reads, writes)
        else:
            P.op(eng, lambda e: e.tensor_copy(out=out, in_=in_), reads=reads, writes=writes)

    def mm(out, pairs, reads, writes):
        def fn(e):
            ins = None
            n = len(pairs)
            for i, (l, r) in enumerate(pairs):
                ins = e.matmul(out, lhsT=l, rhs=r, start=(i == 0), stop=(i == n - 1))
            return ins
        P.op("pe", fn, reads=reads, writes=writes)

    def transposes(outs_ins, reads, writes):
        def fn(e):
            ins = None
            for (o, i) in outs_ins:
                ins = e.transpose(o, i, ident[:])
            return ins
        P.op("pe", fn, reads=list(reads) + ["ident"], writes=writes)

    wslot_ctr = [0]

    def wload(name, r0, kt, c0, ncols):
        i = wslot_ctr[0] % NSLOT
        wslot_ctr[0] += 1
        assert kt * ncols <= SLOT_ELEMS
        view = wring[i][:, 0:kt * ncols].rearrange("p (k n) -> p k n", k=kt)
        src = wb[name][r0:r0 + kt * 128, c0:c0 + ncols].rearrange("(k p) n -> p k n", p=128)
        dma_in(f"w{i}", view, src, writes=[f"wring{i}"], reads=[("wd_" + name, (r0, c0))])
        return view, f"wring{i}"

    def prologue():
        if do_cast:
            pieces = []
            for nm in ("w_xk", "w_xv"):
                pieces += [(nm, 0, 1024, c * 512, 512) for c in range(2)]
            pieces += [("w_dt", 0, 1024, 0, 16)]
            pieces += [("w_tm", 0, 1024, c * 512, 512) for c in range(6)]
            pieces += [("w_fm", 0, 1024, c * 512, 512) for c in range(7)]
            pieces += [("w_out", kh * 1024, 1024, cg * 512, 512) for cg in range(2) for kh in range(2)]
            for nm in ("w_xq", "w_xo"):
                pieces += [(nm, 0, 1024, c * 512, 512) for c in range(2)]
            pieces += [("w_gu", 0, 1024, c * 512, 512) for c in range(11)]
            pieces += [("w_down", k0 * 128, kn * 128, cg * 512, 512) for cg in range(2) for (k0, kn) in ((0, 8), (8, 8), (16, 6))]
            for (nm, r0, nr, c0, ncol) in pieces:
                P.op("pool", (lambda e, a=wb[nm][r0:r0 + nr, c0:c0 + ncol], b_=wf[nm][r0:r0 + nr, c0:c0 + ncol]:
                              e.dma_start(out=a, in_=b_)),
                     reads=[], writes=[("wd_" + nm, (r0, c0))], dma=f"c_{nm}_{r0}_{c0}")
        consts = [(identf, c_ident, "t12"), (U, c_U, "U"), (MGT, c_MGT, "MGT"), (intraT, c_intraT, "intraT"),
                  (xi_b, c_xi, "xi_b"), (zeta16, c_zeta, "zeta16"), (invf, c_invf, "invf"), (gmix, g_mix, "gmix"),
                  (gxat, g_xat, "gxat"), (gffn, g_ffn, "gffn"), (gssd, g_ssd, "gssd"), (gfin, g_fin, "gfin"),
                  (convw, convw_d, "convw"), (convb, convb_d, "convb"), (dtb, dtb_d, "dtb"), (a_b, alog_d, "a_b"),
                  (dsk, dsk_d, "dsk")]
        for (dst, src, key) in consts:
            dma_in("consts", dst[:], src, writes=[key])
        copy("dve", ident[:], identf, ["t12"], ["ident"])
        P.op("pool", lambda e: e.memset(onesf[:], 1.0), writes=["onesf"])
        P.op("pool", lambda e: e.memset(onesb[:], 1.0), writes=["onesb"])
        P.op("pool", lambda e: e.memset(neghalf[:], -0.5), writes=["neghalf"])
        P.op("pool", lambda e: e.memset(negone[:], -1.0), writes=["negone"])
        act_op(a_b[:], a_b[:], AF.Exp, ["a_b"], ["a_b"])
        ts("dve", a_b[:], a_b[:], -1.0, None, ALU.mult, None, ["a_b"], ["a_b"])
        dma_in("consts", wdt[:], wb["w_dt"].rearrange("(k p) n -> p k n", p=128), writes=["wdt"],
               reads=["wd_w_dt"])

    def norms_to_fm(gvec, gkey, src=None):
        stg = [(hs, "hs"), (yr, "yr"), (xs_tm, "xs_tm"), (xdt, "xdt")]
        if src is None:
            srcs = [(h[:, tc, :], ("h", tc)) for tc in range(CH)]
        else:
            srcs = [(src[:, tc, :], ("RB", "p", tc)) for tc in range(CH)]
        sts = []
        for tc in range(CH):
            buf, bk = stg[tc]
            st, sk = small()
            act_op(buf[:], srcs[tc][0], AF.Square, [srcs[tc][1]], [bk, sk], accum_out=st[:, 0:1])
            sts.append((st, sk))
        for tc in range(CH):
            st, sk = sts[tc]
            ts("pool", st[:, 1:2], st[:, 0:1], 1.0 / D, EPS, ALU.mult, ALU.add, [sk], [sk])
            tt("pool", st[:, 2:3], st[:, 1:2], neghalf[:, 0:1], ALU.pow, [sk, "neghalf"], [sk])
        for tc in range(CH):
            buf, bk = stg[tc]
            st, sk = sts[tc]
            act_op(buf[:], srcs[tc][0], AF.Copy, [srcs[tc][1], sk], [bk], scale=st[:, 2:3])
        for tc in range(CH):
            buf, bk = stg[tc]
            p, pk = bank()
            pv = psb(p)
            transposes([(pv[:, f * 128:(f + 1) * 128], buf[:, f * 128:(f + 1) * 128]) for f in range(8)], [bk], [pk])
            tt("dve", hnT[:, :, tc * 128:(tc + 1) * 128], pv[:, 0:1024].rearrange("p (a t) -> p a t", a=8),
               bc(gvec[:, 0:8], 2, [128, 8, 128]), ALU.mult, [pk, gkey], [("hnT", tc)])
            rel(pk)

    def seq_setup(b):
        dma_in("memld", memst[:, :, :], mem_d[b].rearrange("(c p) f -> p c f", p=128), writes=["t12"])
        P.op("pool", lambda e: e.memset(Ss[:], 0.0), writes=["Ss"])
        P.op("pool", lambda e: e.memset(Ssb[:], 0.0), writes=["Ssb"])
        P.op("pool", lambda e: e.memset(Sr[:], 0.0), writes=["Sr"])
        P.op("pool", lambda e: e.memset(Srb[:], 0.0), writes=["Srb"])
        P.op("pool", lambda e: e.memset(hist[:], 0.0), writes=["hist"])
        for mc in range(2):
            copy("act", yr[:], memst[:, mc, :], ["t12"], ["yr"])
            p, pk = bank()
            pv = psb(p)
            transposes([(pv[:, f * 128:(f + 1) * 128], yr[:, f * 128:(f + 1) * 128]) for f in range(8)], ["yr"], [pk])
            copy("dve", memT[:, :, mc * 128:(mc + 1) * 128], pv[:, 0:1024].rearrange("p (a t) -> p a t", a=8),
                 [pk], ["RC"])
            rel(pk)
        for half in range(2):
            w, wk = wload("w_xk", 0, 8, half * 512, 512)
            for j in range(4):
                p, pk = bank()
                mm(p[:, 0:NMEM], [(w[:, kt, j * 128:(j + 1) * 128], memT[:, kt, :]) for kt in range(8)],
                   [wk, "RC"], [pk])
                copy("act", KT[:, half * 4 + j, :], p[:, 0:NMEM], [pk], ["KT"])
                rel(pk)
        for cg in range(2):
            w, wk = wload("w_xv", 0, 8, cg * 512, 512)
            for mc in range(2):
                p, pk = bank()
                mm(p[:, :], [(memT[:, kt, mc * 128:(mc + 1) * 128], w[:, kt, :]) for kt in range(8)],
                   [wk, "RC"], [pk])
                copy("act", Vm[:, mc, cg * 512:(cg + 1) * 512], p[:, :], [pk], ["Vm"])
                rel(pk)

    A_, B_, C_, D_ = RC[:, 0, :], RC[:, 1, :], RC[:, 2, :], RC[:, 3, :]

    def rope_tables(b, blk):
        t0 = blk * T
        RCk = ["RC"]
        dma_in("posld", A_.bitcast(I32), pos_d[b:b + 1, t0:t0 + T].partition_broadcast(128), writes=RCk)
        copy("dve", B_, A_.bitcast(I32), RCk, RCk)
        ts("dve", B_, B_, invf[:, 0:1], None, ALU.mult, None, RCk + ["invf"], RCk)
        ts("dve", C_, B_, 1.0 / TWO_PI, 0.5, ALU.mult, ALU.add, RCk, RCk)
        copy("dve", D_.bitcast(I32), C_, RCk, RCk)
        copy("dve", C_, D_.bitcast(I32), RCk, RCk)
        C1 = 6.28125
        C2 = TWO_PI - C1
        stt(B_, C_, -C1, B_, ALU.mult, ALU.add, RCk, RCk)
        stt(B_, C_, -C2, B_, ALU.mult, ALU.add, RCk, RCk)
        ts("dve", D_, B_, -math.pi, TWO_PI, ALU.is_lt, ALU.mult, RCk, RCk)
        tt("dve", B_, B_, D_, ALU.add, RCk, RCk)
        ts("dve", A_, B_, -math.pi, math.pi, ALU.max, ALU.min, RCk, RCk)
        act_op(sinT[:], A_, AF.Sin, RCk, ["sinT"])
        ts("dve", C_, B_, math.pi / 2, None, ALU.add, None, RCk, RCk)
        ts("dve", D_, C_, math.pi, -TWO_PI, ALU.is_gt, ALU.mult, RCk, RCk)
        tt("dve", C_, C_, D_, ALU.add, RCk, RCk)
        ts("dve", C_, C_, -math.pi, math.pi, ALU.max, ALU.min, RCk, RCk)
        act_op(cosT[:], C_, AF.Sin, RCk, ["cosT"])

    def block(b, blk, nxt, prefetched):
        t0 = blk * T
        if prefetched:
            norms_to_fm(gmix, "gmix", src=xpre)
            for tc in range(CH):
                copy("pool", h[:, tc, :], xpre[:, tc, :], [("RB", "p", tc)], [("h", tc)])
        else:
            for tc in range(CH):
                dma_in(f"xld{tc}", h[:, tc, :], x_d[b, t0 + tc * 128:t0 + (tc + 1) * 128, :], writes=[("h", tc)])
            norms_to_fm(gmix, "gmix")

        dsts = [(zs, ("RB", "m", 0), True), (vt, ("RB", "m", 1), False), (gs, ("RB", "m", 2), True)]
        for wi in range(3):
            dst, dkey, silu = dsts[wi]
            for cg in range(2):
                w, wk = wload("w_tm", 0, 8, wi * 1024 + cg * 512, 512)
                for tc in range(CH):
                    p, pk = bank()
                    mm(p[:, :], [(hnT[:, kt, tc * 128:(tc + 1) * 128], w[:, kt, :]) for kt in range(8)],
                       [wk, ("hnT", tc)], [pk])
                    act_op(dst[:, tc, cg * 512:(cg + 1) * 512], p[:, :], AF.Silu if silu else AF.Copy, [pk], [dkey])
                    rel(pk)
        p, pk = bank()
        for tc in range(CH):
            mm(p[:, tc * 16:(tc + 1) * 16], [(hnT[:, kt, tc * 128:(tc + 1) * 128], wdt[:, kt, :]) for kt in range(8)],
               ["wdt", ("hnT", tc)], [pk])
        X, U1, W1, W2 = spt[0], spt[1], spt[2], spt[3]
        spk = ["spt"]
        tt("dve", X[:].rearrange("p (c h) -> p c h", c=CH), p[:, 0:CH * 16].rearrange("p (c h) -> p c h", c=CH),
           bc(dtb[:, :], 1, [128, CH, 16]), ALU.add, [pk, "dtb"], spk)
        rel(pk)
        def softplus_rest():
            ts("pool", U1[:], X[:], -1.0, 0.0, ALU.mult, ALU.add, spk, spk)
            ts("pool", W2[:], X[:], 1.0, 0.0, ALU.mult, ALU.add, spk, spk)
            ts("pool", U1[:], U1[:], 0.0, None, ALU.min, None, spk, spk)
            ts("pool", W2[:], W2[:], 0.0, None, ALU.min, None, spk, spk)
            tt("pool", U1[:], U1[:], W2[:], ALU.add, spk, spk)
            act_op(U1[:], U1[:], AF.Exp, spk, spk)
            ts("pool", W1[:], U1[:], 1.0, 2.0, ALU.mult, ALU.add, spk, spk)
            tt("pool", W1[:], W1[:], negone[:, :], ALU.pow, spk + ["negone"], spk)
            tt("pool", W1[:], W1[:], U1[:], ALU.mult, spk, spk)
            tt("pool", W2[:], W1[:], W1[:], ALU.mult, spk, spk)
            P.op("pool", lambda e: e.memset(U1[:], 1.0 / 13.0), reads=spk, writes=spk)
            for cc in (1.0 / 11, 1.0 / 9, 1.0 / 7, 1.0 / 5, 1.0 / 3, 1.0):
                tt("pool", U1[:], U1[:], W2[:], ALU.mult, spk, spk)
                ts("pool", U1[:], U1[:], 1.0, cc, ALU.mult, ALU.add, spk, spk)
            tt("pool", U1[:], U1[:], W1[:], ALU.mult, spk, spk)
            ts("pool", X[:], X[:], 0.0, None, ALU.max, None, spk, spk)
            ts("pool", U1[:], U1[:], 2.0, 0.0, ALU.mult, ALU.add, spk, spk)
            dflat = dtt[:, :, :].rearrange("p c h -> p (c h)")
            tt("pool", dflat, U1[:], X[:], ALU.add, spk, ["dtt"])
            tt("pool", da[:, :, :], dtt[:, :, :], bc(a_b[:, :], 1, [128, CH, 16]), ALU.mult, ["dtt", "a_b"], ["da"])


        pend = {}
        for ci in range(7):
            w, wk = wload("w_fm", 0, 8, ci * 512, 512)
            for jj in range(4):
                j = ci * 4 + jj
                p, pk = bank()
                mm(p[:, :], [(w[:, kt, jj * 128:(jj + 1) * 128], hnT[:, kt, :]) for kt in range(8)],
                   [wk, "hnT"], [pk])
                if j < 12:
                    ub = (ubuf[0], ubuf[1], ubuf3)[j % 3]
                    uk = ("ubuf0", "ubuf1", "eseg")[j % 3]
                    cacc = RC[:, j % 4, :]
                    ck = ("RC", "cv", j % 4)
                    copy("act", ub[:, 3:T + 3], p[:, :], [pk], [uk])
                    act_op(cacc, p[:, :], AF.Identity, [pk, "convw", "convb"], [ck], scale=convw[:, j, 3:4],
                           bias=convb[:, j:j + 1])
                    rel(pk)
                    copy("pool", ub[:, 0:3], hist[:, j, :], [("hist", j)], [uk])
                    for k in (0, 1, 2):
                        stt(cacc, ub[:, k:T + k], convw[:, j, k:k + 1], cacc, ALU.mult, ALU.add, [uk, "convw", ck], [ck])
                    copy("pool", hist[:, j, :], ub[:, T:T + 3], [uk], [("hist", j)])
                    if j < 8:
                        act_op(xs_fm[:, j, :], cacc, AF.Silu, [ck], [("RB", "m", 3)])
                    elif j < 10:
                        act_op(Bfm[:, j - 8, :], cacc, AF.Silu, [ck], ["Bfm"])
                    else:
                        act_op(Cfm[:, j - 10, :], cacc, AF.Silu, [ck], ["Cfm"])
                    if j == 11:
                        softplus_rest()
                else:
                    jq = j - 12
                    pend[jq] = (p, pk)
                    if jq % 2 == 1:
                        (p0, pk0), (p1, pk1) = pend[jq - 1], pend[jq]
                        isq = jq < 8
                        dst = q_rot if isq else k_rot
                        dkey = ("RA", "m", 0) if isq else ("RA", "m", 1)
                        te = (jq - 1) % 8
                        rk = [("RC", "rp", i) for i in range(4)]
                        tt("dve", A_, p0[:, :], cosT[:], ALU.mult, [pk0, "cosT"], [rk[0]])
                        tt("dve", B_, p1[:, :], sinT[:], ALU.mult, [pk1, "sinT"], [rk[1]])
                        tt("dve", C_, p1[:, :], cosT[:], ALU.mult, [pk1, "cosT"], [rk[2]])
                        tt("dve", D_, p0[:, :], sinT[:], ALU.mult, [pk0, "sinT"], [rk[3]])
                        rel(pk0, pk1)
                        tt("pool", dst[:, te, :], A_, B_, ALU.subtract, [rk[0], rk[1]], [dkey])
                        tt("pool", dst[:, te + 1, :], C_, D_, ALU.add, [rk[2], rk[3]], [dkey])

        t1 = t12[:, 0, :]
        held = {}
        EB = [(eseg, lambda q4: ("eseg", q4)), (eseg2, lambda q4: ("t12", 1))]

        def rhs(n):
            tt("pool", rhs_cum, bc(da[:, n, :], 2, [128, 16, 128]), bc(U[:, :], 1, [128, 16, 128]), ALU.mult,
               ["da", "U"], ["RC"])

        def A3(n):
            eb, ek = EB[n % 2]
            for q4 in range(4):
                p, pk = bank()
                mm(p[:, :], [(MGT[:, :], rhs_cum[:, q4 * 4:(q4 + 1) * 4, :])], ["MGT", "RC"], [pk])
                act_op(eb[:, q4 * 4:(q4 + 1) * 4, :], p[:, :].rearrange("p (a t) -> p a t", a=4), AF.Exp,
                       [pk], [ek(q4)])
                rel(pk)

        def A1(n):
            cs = slice(n * 128, (n + 1) * 128)
            p, pk = bank()
            pv = psb(p)
            transposes([(pv[:, f * 128:(f + 1) * 128], xs_fm[:, f, cs]) for f in range(8)], [("RB", "m", 3)], [pk])
            tt("dve", xdt[:].rearrange("p (h q) -> p h q", h=16), pv[:, 0:1024].rearrange("p (h q) -> p h q", h=16),
               bc(dtt[:, n, :], 2, [128, 16, 64]), ALU.mult, [pk, "dtt"], ["xdt"])
            tt("dve", xs_tm[:].rearrange("p (h q) -> p h q", h=16), pv[:, 0:1024].rearrange("p (h q) -> p h q", h=16),
               bc(dsk[:, :], 2, [128, 16, 64]), ALU.mult, [pk, "dsk"], ["xs_tm"])
            rel(pk)
            p, pk = bank()
            pv = psb(p)
            transposes([(pv[:, f * 128:(f + 1) * 128], k_rot[:, f, cs]) for f in range(8)], [("RA", "m", 1)], [pk])
            for hh in range(4):
                act_op(kz_tm[:, hh * 256:(hh + 1) * 256], pv[:, hh * 256:(hh + 1) * 256], AF.Copy, [pk, "zeta16"],
                       [("kz_tm", hh)], scale=zeta16[:, hh:hh + 1])
            rel(pk)
            p, pk = bank()
            pv = psb(p)
            transposes([(pv[:, g * 128:(g + 1) * 128], Bfm[:, g, cs]) for g in range(2)], ["Bfm"], [pk])
            copy("act", B_tm[:], pv[:, 0:256], [pk], ["B_tm"])
            rel(pk)
            tt("pool", qx[:, :, :].rearrange("p (h j) t -> p h j t", h=4),
               q_rot[:, :, cs].rearrange("p (h j) t -> p h j t", h=4),
               bc(xi_b[:, :, :], 2, [128, 4, 2, 128]), ALU.mult, [("RA", "m", 0), "xi_b"], ["qx"])

        def A2(n):
            cs = slice(n * 128, (n + 1) * 128)
            p, pk = bank()
            for hh in range(4):
                mm(p[:, hh * 128:(hh + 1) * 128],
                   [(k_rot[:, 2 * hh + j, cs], q_rot[:, 2 * hh + j, cs]) for j in range(2)],
                   [("RA", "m", 0), ("RA", "m", 1)], [pk])
            tt("dve", scm[:, :, :], p[:, :].rearrange("p (a t) -> p a t", a=4), intraT[:, :, :], ALU.mult,
               [pk, "intraT"], ["scm"])
            rel(pk)

        def A5(n):
            cs = slice(n * 128, (n + 1) * 128)
            p, pk = bank()
            for g in range(2):
                mm(p[:, g * 128:(g + 1) * 128], [(Bfm[:, g, cs], Cfm[:, g, cs])], ["Bfm", "Cfm"], [pk])
            tt("dve", CBm[:, :, :], p[:, 0:256].rearrange("p (g t) -> p g t", g=2), bc(U[:, :], 1, [128, 2, 128]),
               ALU.mult, [pk, "U"], ["CBm"])
            rel(pk)
            p3, pk3 = bank()
            mm(p3[:, 0:16], [(U[:, :], da[:, n, :])], ["U", "da"], [pk3])
            mm(p3[:, 16:32], [(MGT[:, :], da[:, n, :])], ["MGT", "da"], [pk3])
            mm(p3[:, 32:48], [(onesf[:, :], da[:, n, :])], ["onesf", "da"], [pk3])
            act_op(e3[:, :], p3[:, 0:48], AF.Exp, [pk3], ["e3"])
            rel(pk3)

        def A6(n):
            eb, ek = EB[n % 2]
            for g in range(2):
                tt("dve", eb[:, g * 8:(g + 1) * 8, :], eb[:, g * 8:(g + 1) * 8, :],
                   bc(CBm[:, g, :], 1, [128, 8, 128]), ALU.mult,
                   [ek(2 * g), ek(2 * g + 1), "CBm"], [ek(2 * g), ek(2 * g + 1)])
            tt("pool", xw[:].rearrange("p (h q) -> p h q", h=16), xdt[:].rearrange("p (h q) -> p h q", h=16),
               bc(e3[:, 16:32], 2, [128, 16, 64]), ALU.mult, ["xdt", "e3"], ["xw"])

        def Bstep(n):
            cs = slice(n * 128, (n + 1) * 128)
            eb, ek = EB[n % 2]
            yd = [bank(), bank()]
            for hh in range(16):
                p, pk = yd[hh // 8]
                c0 = (hh % 8) * 64
                mm(p[:, c0:c0 + 64], [(eb[:, hh, :], xdt[:, hh * 64:(hh + 1) * 64])],
                   [ek(hh // 4), "xdt"], [pk])
            yo = [bank(), bank()]
            for g in range(2):
                p, pk = yo[g]
                mm(p[:, :], [(Cfm[:, g, cs], Ssb[:, g * 512:(g + 1) * 512])], ["Cfm", "Ssb"], [pk])
            dS = [bank(), bank()]
            for g in range(2):
                p, pk = dS[g]
                mm(p[:, :], [(B_tm[:, g * 128:(g + 1) * 128], xw[:, g * 512:(g + 1) * 512])], ["B_tm", "xw"], [pk])
            for g in range(2):
                p, pk = yo[g]
                tt("dve", t1[:, g * 512:(g + 1) * 512].rearrange("p (h q) -> p h q", h=8),
                   p[:, :].rearrange("p (h q) -> p h q", h=8),
                   bc(e3[:, g * 8:(g + 1) * 8], 2, [128, 8, 64]), ALU.mult, [pk, "e3"], [("t12", 0)])
                rel(pk)
            tt("pool", Ss[:].rearrange("p (h q) -> p h q", h=16), Ss[:].rearrange("p (h q) -> p h q", h=16),
               bc(e3[:, 32:48], 2, [128, 16, 64]), ALU.mult, ["Ss", "e3"], ["Ss"])
            for g in range(2):
                p, pk = dS[g]
                tt("dve", Ss[:, g * 512:(g + 1) * 512], Ss[:, g * 512:(g + 1) * 512], p[:, :], ALU.add,
                   [pk, "Ss"], ["Ss"])
                rel(pk)
            copy("act", Ssb[:], Ss[:], ["Ss"], ["Ssb"])
            tt("pool", t1, t1, xs_tm[:], ALU.add, [("t12", 0), "xs_tm"], [("t12", 0)])
            ob = [bank(), bank()]
            for hh in range(4):
                p, pk = ob[hh // 2]
                c0 = (hh % 2) * 256
                mm(p[:, c0:c0 + 256],
                   [(scm[:, hh, :], vt[:, n, hh * 256:(hh + 1) * 256])] +
                   [(qx[:, 2 * hh + j, :], Srb[:, hh, j * 256:(j + 1) * 256]) for j in range(2)],
                   ["scm", ("RB", "m", 1), "qx", "Srb"], [pk])
            dR = [bank(), bank(), bank(), bank()]
            for hh in range(4):
                p, pk = dR[hh]
                for j in range(2):
                    f = 2 * hh + j
                    mm(p[:, j * 256:(j + 1) * 256],
                       [(kz_tm[:, f * 128:(f + 1) * 128], vt[:, n, hh * 256:(hh + 1) * 256])],
                       ["kz_tm", ("RB", "m", 1)], [pk])
            for hh in range(4):
                p, pk = dR[hh]
                cd = (1.0 - 2.0 ** (-5.0 - hh)) ** 128
                stt(Sr[:, hh, :], Sr[:, hh, :], cd, p[:, :], ALU.mult, ALU.add, [pk, "Sr"], [("Sr", hh)])
                rel(pk)
            copy("act", Srb[:, :, :], Sr[:, :, :], ["Sr"], ["Srb"])
            held[n] = (yd, ob)

        def C1(m):
            yd, ob = held[m]
            for g in range(2):
                p, pk = yd[g]
                tt("dve", t1[:, g * 512:(g + 1) * 512], t1[:, g * 512:(g + 1) * 512], p[:, :], ALU.add,
                   [pk, ("t12", 0)], [("t12", 0)])
                rel(pk)

        def C2b(m):
            tt("dve", ysc[:], t1, zs[:, m, :], ALU.mult, [("t12", 0), ("RB", "m", 0)], ["hs"])

        def C3a(m):
            st, sk = small()
            act_op(yr[:], ysc[:], AF.Square, ["hs"], ["yr", sk], accum_out=st[:, 0:1])
            ts("pool", st[:, 1:2], st[:, 0:1], 1.0 / D, EPS, ALU.mult, ALU.add, [sk], [sk])
            tt("pool", rs_ssd[:, m:m + 1], st[:, 1:2], neghalf[:, 0:1], ALU.pow, [sk, "neghalf"], [("rs_ssd", m)])

        def C3b(m):
            cs = slice(m * 128, (m + 1) * 128)
            p, pk = bank()
            pv = psb(p)
            transposes([(pv[:, f * 128:(f + 1) * 128], ysc[:, f * 128:(f + 1) * 128]) for f in range(8)], ["hs"], [pk])
            tt("dve", yT[:, 0:8, cs], pv[:, 0:1024].rearrange("p (a t) -> p a t", a=8),
               bc(gssd[:, 0:8], 2, [128, 8, 128]), ALU.mult, [pk, "gssd"], [("RA", "m", 2)])
            rel(pk)

        cst2 = {}

        def C4a(m):
            yd, ob = held[m]
            st, sk = small()
            for hh in range(4):
                p, pk = ob[hh // 2]
                c0 = (hh % 2) * 256
                act_op(yr[:, hh * 256:(hh + 1) * 256], p[:, c0:c0 + 256], AF.Square, [pk], ["yr", sk],
                       accum_out=st[:, hh:hh + 1])
            st2, sk2 = small()
            ts("pool", st2[:, :], st[:, :], 1.0 / 256, EPS, ALU.mult, ALU.add, [sk], [sk2])
            tt("pool", st2[:, :], st2[:, :], neghalf[:, :], ALU.pow, [sk2, "neghalf"], [sk2])
            cst2[m] = (st2, sk2)

        def C4b(m):
            cs = slice(m * 128, (m + 1) * 128)
            yd, ob = held[m]
            st2, sk2 = cst2[m]
            for hh in range(4):
                p, pk = ob[hh // 2]
                c0 = (hh % 2) * 256
                stt(yr[:, hh * 256:(hh + 1) * 256], p[:, c0:c0 + 256], st2[:, hh:hh + 1],
                    gs[:, m, hh * 256:(hh + 1) * 256], ALU.mult, ALU.mult, [pk, sk2, ("RB", "m", 2)], ["yr"])
            rel(ob[0][1], ob[1][1])
            p, pk = bank()
            pv = psb(p)
            transposes([(pv[:, f * 128:(f + 1) * 128], yr[:, f * 128:(f + 1) * 128]) for f in range(8)], ["yr"], [pk])
            copy("act", yT[:, 8:16, cs], pv[:, 0:1024].rearrange("p (a t) -> p a t", a=8), [pk], [("RA", "m", 3)])
            rel(pk)

        def AC(n, m):
            seq = [("C", C4a), ("A", A1), ("C", C1), ("A", A5), ("C", C2b), ("A", A2), ("C", C4b), ("A", A6),
                   ("C", C3a), ("C", C3b)]
            for kind, fn in seq:
                if kind == "A" and n is not None:
                    fn(n)
                if kind == "C" and m is not None:
                    fn(m)

        rhs(0)
        A3(0)
        AC(0, None)
        for n in range(CH):
            if n + 1 < CH:
                rhs(n + 1)
            Bstep(n)
            if n + 1 < CH:
                A3(n + 1)
                AC(n + 1, n)
            else:
                AC(None, n)

        if nxt is not None:
            rope_tables(*nxt)

        for cg in range(2):
            for kh in range(2):
                w, wk = wload("w_out", kh * 1024, 8, cg * 512, 512)
                for tc in range(CH):
                    p, pk = bank()
                    mm(p[:, :], [(yT[:, kh * 8 + kt, tc * 128:(tc + 1) * 128], w[:, kt, :]) for kt in range(8)],
                       [wk, ("RA", "m", 2 + kh)], [pk])
                    hsl = h[:, tc, cg * 512:(cg + 1) * 512]
                    if kh == 0:
                        stt(hsl, p[:, :], rs_ssd[:, tc:tc + 1], hsl, ALU.mult, ALU.add,
                            [pk, ("h", tc), ("rs_ssd", tc)], [("h", tc)])
                    else:
                        tt("dve", hsl, hsl, p[:, :], ALU.add, [pk, ("h", tc)], [("h", tc)])
                    rel(pk)

        norms_to_fm(gxat, "gxat")
        for half in range(2):
            w, wk = wload("w_xq", 0, 8, half * 512, 512)
            for j in range(4):
                p, pk = bank()
                mm(p[:, :], [(w[:, kt, j * 128:(j + 1) * 128], hnT[:, kt, :]) for kt in range(8)], [wk, "hnT"], [pk])
                copy("act", QT[:, half * 4 + j, :], p[:, :], [pk], [("RB", "x", 0)])
                rel(pk)
        for hh in range(4):
            for mc in range(2):
                p, pk = bank()
                mm(p[:, :], [(KT[:, 2 * hh + j, mc * 128:(mc + 1) * 128], QT[:, 2 * hh + j, :]) for j in range(2)],
                   ["KT", ("RB", "x", 0)], [pk])
                act_op(PT[:, hh * 2 + mc, :], p[:, :], AF.Exp, [pk], [("RB", "x", 1)], scale=1.0 / 16.0)
                rel(pk)
        for hh in range(4):
            p, pk = bank()
            mm(p[:, :], [(onesb[:, :], PT[:, hh * 2 + mc, :]) for mc in range(2)], ["onesb", ("RB", "x", 1)], [pk])
            P.op("dve", (lambda e, p=p, hh=hh: e.reciprocal(out=rinv[:, hh, :], in_=p[:, :])), reads=[pk],
                 writes=[("RB", "x", 2)])
            rel(pk)
            for j in range(2):
                p, pk = bank()
                f = 2 * hh + j
                mm(p[:, :], [(Vm[:, mc, f * 128:(f + 1) * 128], PT[:, hh * 2 + mc, :]) for mc in range(2)],
                   ["Vm", ("RB", "x", 1)], [pk])
                tt("dve", oTn[:, f, :], p[:, :], rinv[:, hh, :], ALU.mult, [pk, ("RB", "x", 2)], [("RB", "x", 3)])
                rel(pk)
        for cg in range(2):
            w, wk = wload("w_xo", 0, 8, cg * 512, 512)
            for tc in range(CH):
                p, pk = bank()
                mm(p[:, :], [(oTn[:, kt, tc * 128:(tc + 1) * 128], w[:, kt, :]) for kt in range(8)],
                   [wk, ("RB", "x", 3)], [pk])
                tt("dve", h[:, tc, cg * 512:(cg + 1) * 512], h[:, tc, cg * 512:(cg + 1) * 512], p[:, :], ALU.add,
                   [pk, ("h", tc)], [("h", tc)])
                rel(pk)

        norms_to_fm(gffn, "gffn")
        for ci in range(11):
            if ci == 4 and nxt is not None:
                nb_, nblk_ = nxt
                for tc in range(CH):
                    dma_in(f"xld{tc}", xpre[:, tc, :],
                           x_d[nb_, nblk_ * T + tc * 128:nblk_ * T + (tc + 1) * 128, :], writes=[("RB", "p", tc)])
            w, wk = wload("w_gu", 0, 8, ci * 512, 512)
            for jj in range(2):
                j = ci * 2 + jj
                pg, pgk = bank()
                pu, puk = bank()
                mm(pg[:, :], [(w[:, kt, jj * 256:jj * 256 + 128], hnT[:, kt, :]) for kt in range(8)], [wk, "hnT"], [pgk])
                mm(pu[:, :], [(w[:, kt, jj * 256 + 128:jj * 256 + 256], hnT[:, kt, :]) for kt in range(8)],
                   [wk, "hnT"], [puk])
                act_op(sg, pg[:, :], AF.Silu, [pgk], ["hs"])
                tt("dve", act[:, j, :], sg, pu[:, :], ALU.mult, [puk, "hs"], [("RA", "f", j)])
                rel(pgk, puk)
        KSPL = [(0, 8), (8, 8), (16, 6)]
        for cg in range(2):
            banks4 = [bank() for _ in range(CH)]
            for ki, (k0, kn) in enumerate(KSPL):
                w, wk = wload("w_down", k0 * 128, kn, cg * 512, 512)
                for tc in range(CH):
                    p, pk = banks4[tc]

                    def fn(e, p=p, w=w, k0=k0, kn=kn, tc=tc, ki=ki):
                        ins = None
                        for kt in range(kn):
                            ins = e.matmul(p[:, :], lhsT=act[:, k0 + kt, tc * 128:(tc + 1) * 128], rhs=w[:, kt, :],
                                           start=(ki == 0 and kt == 0), stop=(ki == 2 and kt == kn - 1))
                        return ins
                    P.op("pe", fn, reads=[wk, ("RA", "f", None)], writes=[pk])
            for tc in range(CH):
                p, pk = banks4[tc]
                tt("dve", h[:, tc, cg * 512:(cg + 1) * 512], h[:, tc, cg * 512:(cg + 1) * 512], p[:, :], ALU.add,
                   [pk, ("h", tc)], [("h", tc)])
                rel(pk)

        sts = []
        for tc in range(CH):
            st, sk = small()
            act_op(hs[:], h[:, tc, :], AF.Square, [("h", tc)], ["hs", sk], accum_out=st[:, 0:1])
            sts.append((st, sk))
        for tc in range(CH):
            st, sk = sts[tc]
            ts("pool", st[:, 1:2], st[:, 0:1], 1.0 / D, EPS, ALU.mult, ALU.add, [sk], [sk])
            tt("pool", st[:, 2:3], st[:, 1:2], neghalf[:, 0:1], ALU.pow, [sk, "neghalf"], [sk])
        for tc in range(CH):
            st, sk = sts[tc]
            par = tc % 2
            stt(t12[:, par, :], h[:, tc, :], st[:, 2:3], gfin[:, :], ALU.mult, ALU.mult,
                [("h", tc), sk, "gfin"], [("t12", par)])
            P.op("pool", (lambda e, par=par, tc=tc: e.dma_start(
                out=out_d[b, t0 + tc * 128:t0 + (tc + 1) * 128, :], in_=t12[:, par, :])),
                reads=[("t12", par)], writes=[("outdram", (b * 100 + blk) * 4 + tc)], dma=f"ost{par}")

    P.unordered.add("consts")
    prologue()
    order = [(b, blk) for b in range(nseq) for blk in range(nblk)]
    for i, (b, blk) in enumerate(order):
        if blk == 0:
            seq_setup(b)
        if i == 0:
            rope_tables(b, blk)
        block(b, blk, order[i + 1] if i + 1 < len(order) else None, i > 0)
    P.final.append(("pool", "ost0"))
    P.final.append(("pool", "ost1"))
    P.emit(nc, es)
    es.close()
    return nc, P


def _consts():
    i = np.arange(128)
    ident = np.eye(128, dtype=np.float32)
    Um = (i[:, None] <= i[None, :]).astype(np.float32)
    MGT = (i[:, None] > i[None, :]).astype(np.float32)
    gam = 1.0 - 2.0 ** (-5.0 - np.arange(4, dtype=np.float64))
    rel = (i[None, :] - i[:, None]).astype(np.float64)
    intraT = np.zeros((128, 4, 128), np.float32)
    xi = np.zeros((128, 4, 128), np.float32)
    zeta = np.zeros((128, 4), np.float32)
    for hh in range(4):
        intraT[:, hh, :] = np.where(rel >= 0, gam[hh] ** np.maximum(rel, 0), 0.0) / 16.0
        xi[:, hh, :] = (gam[hh] ** (i + 1.0))[None, :]
        zeta[:, hh] = gam[hh] ** (127.0 - i) / 16.0
    invf = (1.0 / (10000.0 ** np.linspace(0.0, 1.0, 128, dtype=np.float32))).astype(np.float32)[:, None]
    return dict(c_ident=ident, c_U=Um, c_MGT=MGT, c_intraT=intraT, c_xi=xi, c_zeta=zeta, c_invf=invf)


def prep_shared(inp):
    f = lambda a: np.ascontiguousarray(np.asarray(a, dtype=np.float32))
    w_in = f(inp["w_in"])[0]
    z, xbc, dtw = w_in[:, 0:1024], w_in[:, 1024:2560], w_in[:, 2560:2576]
    q, k, v, g = (w_in[:, 2576 + i * 1024:2576 + (i + 1) * 1024] for i in range(4))
    perm = np.concatenate([np.concatenate([np.arange(0, 256, 2), np.arange(1, 256, 2)]) + hh * 256 for hh in range(4)])
    wg, wu = f(inp["w_gate"])[0], f(inp["w_up"])[0]
    w_gu = np.stack([wg.reshape(1024, 22, 128), wu.reshape(1024, 22, 128)], axis=2).reshape(1024, 5632)
    fm = lambda a: np.ascontiguousarray(f(a).reshape(8, 128).T)
    rep = lambda a: np.ascontiguousarray(np.tile(f(a).reshape(1, -1), (128, 1)))
    cw = f(inp["conv_w"])[0]
    sh = dict(
        w_fm=np.ascontiguousarray(np.concatenate([xbc, q[:, perm], k[:, perm]], axis=1)),
        w_tm=np.ascontiguousarray(np.concatenate([z, v, g], axis=1)),
        w_dt=np.ascontiguousarray(dtw),
        w_out=f(inp["w_out"])[0], w_xq=f(inp["w_xq"])[0], w_xk=f(inp["w_xk"])[0], w_xv=f(inp["w_xv"])[0],
        w_xo=f(inp["w_xo"])[0], w_gu=np.ascontiguousarray(w_gu), w_down=f(inp["w_down"])[0],
        g_mix=fm(inp["norm_mix_g"]), g_xat=fm(inp["norm_xattn_g"]), g_ffn=fm(inp["norm_ffn_g"]),
        g_ssd=fm(inp["ssd_norm_g"]), g_fin=rep(inp["norm_final_g"]),
        convw=np.ascontiguousarray(cw.T.reshape(12, 128, 4).transpose(1, 0, 2)),
        convb=np.ascontiguousarray(f(inp["conv_b"])[0].reshape(12, 128).T),
        dtb=rep(inp["dt_bias"]), alog=rep(inp["a_log"]), dsk=rep(inp["d_skip"]),
    )
    sh.update(_consts())
    return sh


def kernel(**inputs):
    sh = prep_shared(inputs)
    x = np.asarray(inputs["x"], dtype=np.float32)
    mem = np.asarray(inputs["mem"], dtype=np.float32)
    pos = np.asarray(inputs["positions"], dtype=np.int32)
    nseq = BATCH // NCORES
    nc, _ = build_program(nseq=nseq, nblk=SEQ // T)
    in_maps = []
    for c in range(NCORES):
        m = dict(sh)
        m["x"] = np.ascontiguousarray(x[c * nseq:(c + 1) * nseq])
        m["mem"] = np.ascontiguousarray(mem[c * nseq:(c + 1) * nseq])
        m["pos"] = np.ascontiguousarray(pos[c * nseq:(c + 1) * nseq])
        in_maps.append(m)
    res = run_bass_kernel_spmd(nc, in_maps, core_ids=list(range(NCORES)))
    return np.concatenate([np.asarray(r["out"]) for r in res.results], axis=0).astype(np.float32)
```

```python
import math
import numpy as np
from contextlib import ExitStack

import concourse.bass as bass
import concourse.mybir as mybir
from concourse.bass_utils import run_bass_kernel_spmd

F32 = mybir.dt.float32
BF16 = mybir.dt.bfloat16
I32 = mybir.dt.int32
AF = mybir.ActivationFunctionType
ALU = mybir.AluOpType

NCORES = 8
D = 1024
SEQ = 2048
BATCH = 32
NMEM = 256
DFF = 2816
T = 512
CH = 4
EPS = 1e-6
EPOCH = 12000
NSLOT = 4
SLOT_ELEMS = 4096
TWO_PI = 2.0 * math.pi


class Prog:
    ENG = ("pe", "act", "dve", "pool", "sp")

    def __init__(self):
        self.streams = {e: [] for e in self.ENG}
        self.cnt = {}
        self.state = {}
        self.waited = {e: {} for e in self.ENG}
        self.dma_streams = []
        self.final = []
        self.unordered = set()

    def _conf(self, key):
        page, ns, sub = key
        d = self.state.setdefault(page, {})
        out = []
        for (ns2, sub2), st in d.items():
            if ns2 != ns or sub is None or sub2 is None or sub2 == sub:
                out.append(st)
        return out

    def _get(self, key):
        page, ns, sub = key
        d = self.state.setdefault(page, {})
        if (ns, sub) not in d:
            d[(ns, sub)] = [None, {}, {}]
        return d[(ns, sub)]

    @staticmethod
    def _norm(k):
        if isinstance(k, str):
            return (k, "", None)
        if len(k) == 2:
            return (k[0], "", k[1])
        return k

    def op(self, eng, fn, reads=(), writes=(), dma=None):
        reads = [self._norm(k) for k in reads]
        writes = [self._norm(k) for k in writes]
        xreads = [k for k in reads if k[0].startswith("ps")]
        reads = [k for k in reads if not k[0].startswith("ps")]
        veng = dma if dma is not None else eng
        deps = {}

        def add(dep):
            if dep is None:
                return
            v, i = dep
            if deps.get(v, 0) < i:
                deps[v] = i

        for r in reads:
            for st in self._conf(r):
                add(st[0])
        for x in xreads:
            for st in self._conf(x):
                add(st[0])
                for v, i in st[2].items():
                    if v != veng:
                        add((v, i))
        for w in writes:
            for st in self._conf(w):
                add(st[0])
                for v, i in st[1].items():
                    add((v, i))
                for v, i in st[2].items():
                    add((v, i))
        waits = []
        for v, i in deps.items():
            if v == "pe" and eng == "pe":
                continue
            if self.waited[eng].get(v, 0) >= i:
                continue
            self.waited[eng][v] = i
            waits.append((v, i))
        if dma is not None and dma not in self.cnt:
            self.dma_streams.append(dma)
        self.cnt[veng] = self.cnt.get(veng, 0) + 1
        idx = self.cnt[veng]
        for r in reads:
            st = self._get(r)
            if st[1].get(veng, 0) < idx:
                st[1][veng] = idx
        for x in xreads:
            st = self._get(x)
            if st[2].get(veng, 0) < idx:
                st[2][veng] = idx
        for w in writes:
            page, ns, sub = w
            if sub is None:
                d = self.state.setdefault(page, {})
                for k2 in [k2 for k2 in d if k2[0] == ns and k2[1] is not None]:
                    del d[k2]
            st = self._get(w)
            st[0] = (veng, idx)
            st[1] = {}
            st[2] = {}
        self.streams[eng].append((waits, fn, veng, idx))

    def emit(self, nc, es):
        sems = {}
        for e in ("pe", "act", "dve", "pool"):
            n = self.cnt.get(e, 0)
            ne = max(1, (n + EPOCH - 1) // EPOCH)
            sems[e] = [es.enter_context(nc.semaphore(f"s_{e}{k}")) for k in range(ne)]
        for d in self.dma_streams:
            sems[d] = [es.enter_context(nc.semaphore(f"d_{d}"))]

        def semval(v, i):
            if v in ("pe", "act", "dve", "pool"):
                return sems[v][(i - 1) // EPOCH], (i - 1) % EPOCH + 1
            if v in self.unordered:
                return sems[v][0], 16 * self.cnt[v]
            return sems[v][0], 16 * i

        block = es.enter_context(nc.Block())

        def runner(name):
            def f(e):
                for (waits, fn, veng, idx) in self.streams[name]:
                    for (v, i) in waits:
                        s, val = semval(v, i)
                        e.wait_ge(s, val)
                    ins = fn(e)
                    s, val = semval(veng, idx)
                    if veng in ("pe", "act", "dve", "pool"):
                        ins.then_inc(s, 1)
                    else:
                        ins.then_inc(s, 16)
                for (fe, d) in self.final:
                    if fe == name:
                        s, val = semval(d, self.cnt[d])
                        e.wait_ge(s, val)
            return f

        block.tensor(runner("pe"))
        block.scalar(runner("act"))
        block.vector(runner("dve"))
        block.gpsimd(runner("pool"))
        block.sync(runner("sp"))


def bc(ap, axis, shape):
    return ap.unsqueeze(axis).to_broadcast(shape)


def build_program(nseq=4, nblk=4, do_cast=True):
    nc = bass.Bass("TRN2", target_bir_lowering=False)
    P = Prog()
    es = ExitStack()
    L = nblk * T

    def din(name, shape, dt=F32):
        return nc.dram_tensor(name, list(shape), dt, kind="ExternalInput").ap()

    x_d = din("x", [nseq, L, D])
    mem_d = din("mem", [nseq, NMEM, D])
    pos_d = din("pos", [nseq, L], I32)
    out_d = nc.dram_tensor("out", [nseq, L, D], F32, kind="ExternalOutput").ap()
    WSPEC = {
        "w_fm": (1024, 3584), "w_tm": (1024, 3072), "w_out": (2048, 1024), "w_xq": (1024, 1024),
        "w_xk": (1024, 1024), "w_xv": (1024, 1024), "w_xo": (1024, 1024), "w_gu": (1024, 5632),
        "w_down": (2816, 1024), "w_dt": (1024, 16),
    }
    wf = {k: din(k, v) for k, v in WSPEC.items()}
    wb = {k: nc.dram_tensor(k + "_b", list(v), BF16, kind="Internal").ap() for k, v in WSPEC.items()}
    c_ident = din("c_ident", [128, 128])
    c_U = din("c_U", [128, 128])
    c_MGT = din("c_MGT", [128, 128])
    c_intraT = din("c_intraT", [128, 4, 128])
    c_xi = din("c_xi", [128, 4, 128])
    c_zeta = din("c_zeta", [128, 4])
    c_invf = din("c_invf", [128, 1])
    g_mix = din("g_mix", [128, 8])
    g_xat = din("g_xat", [128, 8])
    g_ffn = din("g_ffn", [128, 8])
    g_ssd = din("g_ssd", [128, 8])
    g_fin = din("g_fin", [128, 1024])
    convw_d = din("convw", [128, 12, 4])
    convb_d = din("convb", [128, 12])
    dtb_d = din("dtb", [128, 16])
    alog_d = din("alog", [128, 16])
    dsk_d = din("dsk", [128, 16])

    def sb(name, shape, dt=F32):
        return es.enter_context(nc.sbuf_tensor(name, list(shape), dt))

    ident = sb("ident", [128, 128], BF16)
    U = sb("U", [128, 128])
    MGT = sb("MGT", [128, 128])
    onesf = sb("onesf", [128, 128])
    onesb = sb("onesb", [128, 128], BF16)
    intraT = sb("intraT", [128, 4, 128])
    xi_b = sb("xi_b", [128, 4, 128])
    zeta16 = sb("zeta16", [128, 4])
    invf = sb("invf", [128, 1])
    gmix = sb("gmix", [128, 8])
    gxat = sb("gxat", [128, 8])
    gffn = sb("gffn", [128, 8])
    gssd = sb("gssd", [128, 8])
    gfin = sb("gfin", [128, 1024])
    convw = sb("convw_s", [128, 12, 4])
    convb = sb("convb_s", [128, 12])
    dtb = sb("dtb_s", [128, 16])
    a_b = sb("a_b", [128, 16])
    dsk = sb("dsk_s", [128, 16])
    neghalf = sb("neghalf", [128, 4])
    negone = sb("negone", [128, CH * 16])
    wdt = sb("wdt", [128, 8, 16], BF16)
    wring = [sb(f"wring{i}", [128, SLOT_ELEMS], BF16) for i in range(NSLOT)]
    h = sb("h", [128, CH, D])
    hs = sb("hs", [128, D], BF16)
    hnT = sb("hnT", [128, 8, T], BF16)
    ubuf = [sb(f"ubuf{i}", [128, T + 3]) for i in range(2)]
    hist = sb("hist", [128, 12, 3])
    RA = sb("RA", [128, 4, 4096], BF16)
    RB = sb("RB", [128, 4, 4096], BF16)
    RC = sb("RC", [128, 4, 512])
    Bfm = sb("Bfm", [128, 2, T], BF16)
    Cfm = sb("Cfm", [128, 2, T], BF16)
    cosT = sb("cosT", [128, T])
    sinT = sb("sinT", [128, T])
    xs_tm = sb("xs_tm", [128, D], BF16)
    xdt = sb("xdt", [128, D], BF16)
    kz_tm = sb("kz_tm", [128, D], BF16)
    B_tm = sb("B_tm", [128, 256], BF16)
    qx = sb("qx", [128, 8, 128], BF16)
    eseg = sb("eseg", [128, 16, 128], BF16)
    CBm = sb("CBm", [128, 2, 128], BF16)
    xw = sb("xw", [128, D], BF16)
    t12 = sb("t12", [128, 2, D])
    Ss = sb("Ss", [128, D])
    Ssb = sb("Ssb", [128, D], BF16)
    Sr = sb("Sr", [128, 4, 512])
    Srb = sb("Srb", [128, 4, 512], BF16)
    scm = sb("scm", [128, 4, 128], BF16)
    yr = sb("yr", [128, D], BF16)
    KT = sb("KT", [128, 8, NMEM], BF16)
    Vm = sb("Vm", [128, 2, D], BF16)
    dtt = sb("dtt", [128, CH, 16])
    da = sb("da", [128, CH, 16])
    spt = [sb(f"spt{i}", [128, CH * 16]) for i in range(4)]
    e3 = sb("e3", [128, 48])
    rs_ssd = sb("rs_ssd", [128, CH])
    smalls = [sb(f"sm{i}", [128, 4]) for i in range(8)]
    sm_ctr = [0]

    def small():
        sm_ctr[0] += 1
        i = sm_ctr[0] % len(smalls)
        return smalls[i], f"sm{i}"

    q_rot = RA[:, 0, :].rearrange("p (a t) -> p a t", a=8)
    k_rot = RA[:, 1, :].rearrange("p (a t) -> p a t", a=8)
    yT = RA[:, 2:4, :].rearrange("p c (a t) -> p (c a) t", a=8)
    RAflat = RA[:, :, :].rearrange("p c e -> p (c e)")
    act = RAflat[:, 0:22 * T].rearrange("p (a t) -> p a t", a=22)
    zs = RB[:, 0, :].rearrange("p (c f) -> p c f", c=CH)
    vt = RB[:, 1, :].rearrange("p (c f) -> p c f", c=CH)
    gs = RB[:, 2, :].rearrange("p (c f) -> p c f", c=CH)
    xs_fm = RB[:, 3, :].rearrange("p (a t) -> p a t", a=8)
    QT = RB[:, 0, :].rearrange("p (a t) -> p a t", a=8)
    PT = RB[:, 1, :].rearrange("p (a t) -> p a t", a=8)
    rinv = RB[:, 2, :].bitcast(F32).rearrange("p (a t) -> p a t", a=4)
    oTn = RB[:, 3, :].rearrange("p (a t) -> p a t", a=8)
    ubuf3 = eseg[:, :, :].rearrange("p a t -> p (a t)").bitcast(F32)[:, 0:T + 3]
    eseg2 = t12[:, 1, :].bitcast(BF16).rearrange("p (a t) -> p a t", a=16)
    xpre = RB[:, 0:2, :].bitcast(F32).rearrange("p a (c f) -> p (a c) f", f=D)
    rhs_cum = RC[:, :, :].rearrange("p a (b t) -> p (a b) t", b=4)
    memT = RC[:, :, :].bitcast(BF16)[:, 0:2, :].rearrange("p a (k m) -> p (a k) m", m=NMEM)
    memst = t12
    ysc = hs
    sg = hs[:, 0:T]
    identf = t12[:, 0, 0:128]

    PS = [es.enter_context(nc.psum_tensor(f"ps{i}", [128, 512], F32)) for i in range(8)]
    free_banks = list(range(8))

    def bank():
        assert free_banks, "PSUM exhausted"
        i = free_banks.pop(0)
        return PS[i], f"ps{i}"

    def rel(*pks):
        for pk in pks:
            i = int(pk[2:])
            assert i not in free_banks
            free_banks.append(i)

    def psb(p):
        return p[:].bitcast(BF16)

    def dma_in(stream, out_ap, in_ap, writes, reads=()):
        P.op("sp", lambda e: e.dma_start(out=out_ap, in_=in_ap), reads=reads, writes=writes, dma=stream)

    def act_op(out, in_, func, reads, writes, **kw):
        P.op("act", lambda e: e.activation(out=out, in_=in_, func=func, **kw), reads=reads, writes=writes)

    def tt(eng, out, in0, in1, op, reads, writes):
        P.op(eng, lambda e: e.tensor_tensor(out=out, in0=in0, in1=in1, op=op), reads=reads, writes=writes)

    def ts(eng, out, in0, s1, s2, op0, op1, reads, writes):
        if s2 is None:
            P.op(eng, lambda e: e.tensor_scalar(out=out, in0=in0, scalar1=s1, scalar2=None, op0=op0),
                 reads=reads, writes=writes)
        else:
            P.op(eng, lambda e: e.tensor_scalar(out=out, in0=in0, scalar1=s1, scalar2=s2, op0=op0, op1=op1),
                 reads=reads, writes=writes)

    def stt(out, in0, scalar, in1, op0, op1, reads, writes):
        P.op("dve", lambda e: e.scalar_tensor_tensor(out=out, in0=in0, scalar=scalar, in1=in1, op0=op0, op1=op1),
             reads=reads, writes=writes)

    def copy(eng, out, in_, reads, writes):
        if eng == "act":
            act_op(out, in_, AF.Copy, reads, writes)
        else:
            P.op(eng, lambda e: e.tensor_copy(out=out, in_=in_), reads=reads, writes=writes)

    def mm(out, pairs, reads, writes):
        def fn(e):
            ins = None
            n = len(pairs)
            for i, (l, r) in enumerate(pairs):
                ins = e.matmul(out, lhsT=l, rhs=r, start=(i == 0), stop=(i == n - 1))
            return ins
        P.op("pe", fn, reads=reads, writes=writes)

    def transposes(outs_ins, reads, writes):
        def fn(e):
            ins = None
            for (o, i) in outs_ins:
                ins = e.transpose(o, i, ident[:])
            return ins
        P.op("pe", fn, reads=list(reads) + ["ident"], writes=writes)

    wslot_ctr = [0]

    def wload(name, r0, kt, c0, ncols):
        i = wslot_ctr[0] % NSLOT
        wslot_ctr[0] += 1
        assert kt * ncols <= SLOT_ELEMS
        view = wring[i][:, 0:kt * ncols].rearrange("p (k n) -> p k n", k=kt)
        src = wb[name][r0:r0 + kt * 128, c0:c0 + ncols].rearrange("(k p) n -> p k n", p=128)
        dma_in(f"w{i}", view, src, writes=[f"wring{i}"], reads=[("wd_" + name, (r0, c0))])
        return view, f"wring{i}"

    def prologue():
        if do_cast:
            pieces = []
            for nm in ("w_xk", "w_xv"):
                pieces += [(nm, 0, 1024, c * 512, 512) for c in range(2)]
            pieces += [("w_dt", 0, 1024, 0, 16)]
            pieces += [("w_tm", 0, 1024, c * 512, 512) for c in range(6)]
            pieces += [("w_fm", 0, 1024, c * 512, 512) for c in range(7)]
            pieces += [("w_out", kh * 1024, 1024, cg * 512, 512) for cg in range(2) for kh in range(2)]
            for nm in ("w_xq", "w_xo"):
                pieces += [(nm, 0, 1024, c * 512, 512) for c in range(2)]
            pieces += [("w_gu", 0, 1024, c * 512, 512) for c in range(11)]
            pieces += [("w_down", k0 * 128, kn * 128, cg * 512, 512) for cg in range(2) for (k0, kn) in ((0, 8), (8, 8), (16, 6))]
            for (nm, r0, nr, c0, ncol) in pieces:
                P.op("pool", (lambda e, a=wb[nm][r0:r0 + nr, c0:c0 + ncol], b_=wf[nm][r0:r0 + nr, c0:c0 + ncol]:
                              e.dma_start(out=a, in_=b_)),
                     reads=[], writes=[("wd_" + nm, (r0, c0))], dma=f"c_{nm}_{r0}_{c0}")
        consts = [(identf, c_ident, "t12"), (U, c_U, "U"), (MGT, c_MGT, "MGT"), (intraT, c_intraT, "intraT"),
                  (xi_b, c_xi, "xi_b"), (zeta16, c_zeta, "zeta16"), (invf, c_invf, "invf"), (gmix, g_mix, "gmix"),
                  (gxat, g_xat, "gxat"), (gffn, g_ffn, "gffn"), (gssd, g_ssd, "gssd"), (gfin, g_fin, "gfin"),
                  (convw, convw_d, "convw"), (convb, convb_d, "convb"), (dtb, dtb_d, "dtb"), (a_b, alog_d, "a_b"),
                  (dsk, dsk_d, "dsk")]
        for (dst, src, key) in consts:
            dma_in("consts", dst[:], src, writes=[key])
        copy("dve", ident[:], identf, ["t12"], ["ident"])
        P.op("pool", lambda e: e.memset(onesf[:], 1.0), writes=["onesf"])
        P.op("pool", lambda e: e.memset(onesb[:], 1.0), writes=["onesb"])
        P.op("pool", lambda e: e.memset(neghalf[:], -0.5), writes=["neghalf"])
        P.op("pool", lambda e: e.memset(negone[:], -1.0), writes=["negone"])
        act_op(a_b[:], a_b[:], AF.Exp, ["a_b"], ["a_b"])
        ts("dve", a_b[:], a_b[:], -1.0, None, ALU.mult, None, ["a_b"], ["a_b"])
        dma_in("consts", wdt[:], wb["w_dt"].rearrange("(k p) n -> p k n", p=128), writes=["wdt"],
               reads=["wd_w_dt"])

    def norms_to_fm(gvec, gkey, src=None):
        stg = [(hs, "hs"), (yr, "yr"), (xs_tm, "xs_tm"), (xdt, "xdt")]
        if src is None:
            srcs = [(h[:, tc, :], ("h", tc)) for tc in range(CH)]
        else:
            srcs = [(src[:, tc, :], ("RB", "p", tc)) for tc in range(CH)]
        sts = []
        for tc in range(CH):
            buf, bk = stg[tc]
            st, sk = small()
            act_op(buf[:], srcs[tc][0], AF.Square, [srcs[tc][1]], [bk, sk], accum_out=st[:, 0:1])
            sts.append((st, sk))
        for tc in range(CH):
            st, sk = sts[tc]
            ts("pool", st[:, 1:2], st[:, 0:1], 1.0 / D, EPS, ALU.mult, ALU.add, [sk], [sk])
            tt("pool", st[:, 2:3], st[:, 1:2], neghalf[:, 0:1], ALU.pow, [sk, "neghalf"], [sk])
        for tc in range(CH):
            buf, bk = stg[tc]
            st, sk = sts[tc]
            act_op(buf[:], srcs[tc][0], AF.Copy, [srcs[tc][1], sk], [bk], scale=st[:, 2:3])
        for tc in range(CH):
            buf, bk = stg[tc]
            p, pk = bank()
            pv = psb(p)
            transposes([(pv[:, f * 128:(f + 1) * 128], buf[:, f * 128:(f + 1) * 128]) for f in range(8)], [bk], [pk])
            tt("dve", hnT[:, :, tc * 128:(tc + 1) * 128], pv[:, 0:1024].rearrange("p (a t) -> p a t", a=8),
               bc(gvec[:, 0:8], 2, [128, 8, 128]), ALU.mult, [pk, gkey], [("hnT", tc)])
            rel(pk)

    def seq_setup(b):
        dma_in("memld", memst[:, :, :], mem_d[b].rearrange("(c p) f -> p c f", p=128), writes=["t12"])
        P.op("pool", lambda e: e.memset(Ss[:], 0.0), writes=["Ss"])
        P.op("pool", lambda e: e.memset(Ssb[:], 0.0), writes=["Ssb"])
        P.op("pool", lambda e: e.memset(Sr[:], 0.0), writes=["Sr"])
        P.op("pool", lambda e: e.memset(Srb[:], 0.0), writes=["Srb"])
        P.op("pool", lambda e: e.memset(hist[:], 0.0), writes=["hist"])
        for mc in range(2):
            copy("act", yr[:], memst[:, mc, :], ["t12"], ["yr"])
            p, pk = bank()
            pv = psb(p)
            transposes([(pv[:, f * 128:(f + 1) * 128], yr[:, f * 128:(f + 1) * 128]) for f in range(8)], ["yr"], [pk])
            copy("dve", memT[:, :, mc * 128:(mc + 1) * 128], pv[:, 0:1024].rearrange("p (a t) -> p a t", a=8),
                 [pk], ["RC"])
            rel(pk)
        for half in range(2):
            w, wk = wload("w_xk", 0, 8, half * 512, 512)
            for j in range(4):
                p, pk = bank()
                mm(p[:, 0:NMEM], [(w[:, kt, j * 128:(j + 1) * 128], memT[:, kt, :]) for kt in range(8)],
                   [wk, "RC"], [pk])
                copy("act", KT[:, half * 4 + j, :], p[:, 0:NMEM], [pk], ["KT"])
                rel(pk)
        for cg in range(2):
            w, wk = wload("w_xv", 0, 8, cg * 512, 512)
            for mc in range(2):
                p, pk = bank()
                mm(p[:, :], [(memT[:, kt, mc * 128:(mc + 1) * 128], w[:, kt, :]) for kt in range(8)],
                   [wk, "RC"], [pk])
                copy("act", Vm[:, mc, cg * 512:(cg + 1) * 512], p[:, :], [pk], ["Vm"])
                rel(pk)

    A_, B_, C_, D_ = RC[:, 0, :], RC[:, 1, :], RC[:, 2, :], RC[:, 3, :]

    def rope_tables(b, blk):
        t0 = blk * T
        RCk = ["RC"]
        dma_in("posld", A_.bitcast(I32), pos_d[b:b + 1, t0:t0 + T].partition_broadcast(128), writes=RCk)
        copy("dve", B_, A_.bitcast(I32), RCk, RCk)
        ts("dve", B_, B_, invf[:, 0:1], None, ALU.mult, None, RCk + ["invf"], RCk)
        ts("dve", C_, B_, 1.0 / TWO_PI, 0.5, ALU.mult, ALU.add, RCk, RCk)
        copy("dve", D_.bitcast(I32), C_, RCk, RCk)
        copy("dve", C_, D_.bitcast(I32), RCk, RCk)
        C1 = 6.28125
        C2 = TWO_PI - C1
        stt(B_, C_, -C1, B_, ALU.mult, ALU.add, RCk, RCk)
        stt(B_, C_, -C2, B_, ALU.mult, ALU.add, RCk, RCk)
        ts("dve", D_, B_, -math.pi, TWO_PI, ALU.is_lt, ALU.mult, RCk, RCk)
        tt("dve", B_, B_, D_, ALU.add, RCk, RCk)
        ts("dve", A_, B_, -math.pi, math.pi, ALU.max, ALU.min, RCk, RCk)
        act_op(sinT[:], A_, AF.Sin, RCk, ["sinT"])
        ts("dve", C_, B_, math.pi / 2, None, ALU.add, None, RCk, RCk)
        ts("dve", D_, C_, math.pi, -TWO_PI, ALU.is_gt, ALU.mult, RCk, RCk)
        tt("dve", C_, C_, D_, ALU.add, RCk, RCk)
        ts("dve", C_, C_, -math.pi, math.pi, ALU.max, ALU.min, RCk, RCk)
        act_op(cosT[:], C_, AF.Sin, RCk, ["cosT"])

    def block(b, blk, nxt, prefetched):
        t0 = blk * T
        if prefetched:
            for tc in range(CH):
                copy("pool", h[:, tc, :], xpre[:, tc, :], [("RB", "p", tc)], [("h", tc)])
        else:
            for tc in range(CH):
                dma_in(f"xld{tc}", h[:, tc, :], x_d[b, t0 + tc * 128:t0 + (tc + 1) * 128, :], writes=[("h", tc)])
            norms_to_fm(gmix, "gmix")

        dsts = [(zs, ("RB", "m", 0), True), (vt, ("RB", "m", 1), False), (gs, ("RB", "m", 2), True)]
        for wi in range(3):
            dst, dkey, silu = dsts[wi]
            for cg in range(2):
                w, wk = wload("w_tm", 0, 8, wi * 1024 + cg * 512, 512)
                for tc in range(CH):
                    p, pk = bank()
                    mm(p[:, :], [(hnT[:, kt, tc * 128:(tc + 1) * 128], w[:, kt, :]) for kt in range(8)],
                       [wk, ("hnT", tc)], [pk])
                    act_op(dst[:, tc, cg * 512:(cg + 1) * 512], p[:, :], AF.Silu if silu else AF.Copy, [pk], [dkey])
                    rel(pk)
        p, pk = bank()
        for tc in range(CH):
            mm(p[:, tc * 16:(tc + 1) * 16], [(hnT[:, kt, tc * 128:(tc + 1) * 128], wdt[:, kt, :]) for kt in range(8)],
               ["wdt", ("hnT", tc)], [pk])
        X, U1, W1, W2 = spt[0], spt[1], spt[2], spt[3]
        spk = ["spt"]
        tt("dve", X[:].rearrange("p (c h) -> p c h", c=CH), p[:, 0:CH * 16].rearrange("p (c h) -> p c h", c=CH),
           bc(dtb[:, :], 1, [128, CH, 16]), ALU.add, [pk, "dtb"], spk)
        rel(pk)
        def softplus_rest():
            ts("pool", U1[:], X[:], -1.0, 0.0, ALU.mult, ALU.add, spk, spk)
            ts("pool", W2[:], X[:], 1.0, 0.0, ALU.mult, ALU.add, spk, spk)
            ts("pool", U1[:], U1[:], 0.0, None, ALU.min, None, spk, spk)
            ts("pool", W2[:], W2[:], 0.0, None, ALU.min, None, spk, spk)
            tt("pool", U1[:], U1[:], W2[:], ALU.add, spk, spk)
            act_op(U1[:], U1[:], AF.Exp, spk, spk)
            ts("pool", W1[:], U1[:], 1.0, 2.0, ALU.mult, ALU.add, spk, spk)
            tt("pool", W1[:], W1[:], negone[:, :], ALU.pow, spk + ["negone"], spk)
            tt("pool", W1[:], W1[:], U1[:], ALU.mult, spk, spk)
            tt("pool", W2[:], W1[:], W1[:], ALU.mult, spk, spk)
            P.op("pool", lambda e: e.memset(U1[:], 1.0 / 9.0), reads=spk, writes=spk)
            for cc in (1.0 / 7, 1.0 / 5, 1.0 / 3, 1.0):
                tt("pool", U1[:], U1[:], W2[:], ALU.mult, spk, spk)
                ts("pool", U1[:], U1[:], 1.0, cc, ALU.mult, ALU.add, spk, spk)
            tt("pool", U1[:], U1[:], W1[:], ALU.mult, spk, spk)
            ts("pool", X[:], X[:], 0.0, None, ALU.max, None, spk, spk)
            ts("pool", U1[:], U1[:], 2.0, 0.0, ALU.mult, ALU.add, spk, spk)
            dflat = dtt[:, :, :].rearrange("p c h -> p (c h)")
            tt("pool", dflat, U1[:], X[:], ALU.add, spk, ["dtt"])
            tt("pool", da[:, :, :], dtt[:, :, :], bc(a_b[:, :], 1, [128, CH, 16]), ALU.mult, ["dtt", "a_b"], ["da"])


        softplus_rest()

        pend = {}
        for ci in range(7):
            w, wk = wload("w_fm", 0, 8, ci * 512, 512)
            for jj in range(4):
                j = ci * 4 + jj
                p, pk = bank()
                mm(p[:, :], [(w[:, kt, jj * 128:(jj + 1) * 128], hnT[:, kt, :]) for kt in range(8)],
                   [wk, "hnT"], [pk])
                if j < 12:
                    ub = (ubuf[0], ubuf[1], ubuf3)[j % 3]
                    uk = ("ubuf0", "ubuf1", "eseg")[j % 3]
                    cacc = RC[:, j % 4, :]
                    ck = ("RC", "cv", j % 4)
                    copy("act", ub[:, 3:T + 3], p[:, :], [pk], [uk])
                    act_op(cacc, p[:, :], AF.Identity, [pk, "convw", "convb"], [ck], scale=convw[:, j, 3:4],
                           bias=convb[:, j:j + 1])
                    rel(pk)
                    copy("act", ub[:, 0:3], hist[:, j, :], [("hist", j)], [uk])
                    for k in (0, 1, 2):
                        stt(cacc, ub[:, k:T + k], convw[:, j, k:k + 1], cacc, ALU.mult, ALU.add, [uk, "convw", ck], [ck])
                    copy("act", hist[:, j, :], ub[:, T:T + 3], [uk], [("hist", j)])
                    if j < 8:
                        act_op(xs_fm[:, j, :], cacc, AF.Silu, [ck], [("RB", "m", 3)])
                    elif j < 10:
                        act_op(Bfm[:, j - 8, :], cacc, AF.Silu, [ck], ["Bfm"])
                    else:
                        act_op(Cfm[:, j - 10, :], cacc, AF.Silu, [ck], ["Cfm"])
                else:
                    jq = j - 12
                    pend[jq] = (p, pk)
                    if jq % 2 == 1:
                        (p0, pk0), (p1, pk1) = pend[jq - 1], pend[jq]
                        isq = jq < 8
                        dst = q_rot if isq else k_rot
                        dkey = ("RA", "m", 0) if isq else ("RA", "m", 1)
                        te = (jq - 1) % 8
                        rk = [("RC", "rp", i) for i in range(4)]
                        tt("dve", A_, p0[:, :], cosT[:], ALU.mult, [pk0, "cosT"], [rk[0]])
                        tt("dve", B_, p1[:, :], sinT[:], ALU.mult, [pk1, "sinT"], [rk[1]])
                        tt("dve", C_, p1[:, :], cosT[:], ALU.mult, [pk1, "cosT"], [rk[2]])
                        tt("dve", D_, p0[:, :], sinT[:], ALU.mult, [pk0, "sinT"], [rk[3]])
                        rel(pk0, pk1)
                        tt("pool", dst[:, te, :], A_, B_, ALU.subtract, [rk[0], rk[1]], [dkey])
                        tt("pool", dst[:, te + 1, :], C_, D_, ALU.add, [rk[2], rk[3]], [dkey])

        t1 = t12[:, 0, :]
        held = {}
        EB = [(eseg, lambda q4: ("eseg", q4)), (eseg2, lambda q4: ("t12", 1))]

        def rhs(n):
            tt("pool", rhs_cum, bc(da[:, n, :], 2, [128, 16, 128]), bc(U[:, :], 1, [128, 16, 128]), ALU.mult,
               ["da", "U"], ["RC"])

        def A3(n):
            eb, ek = EB[n % 2]
            for q4 in range(4):
                p, pk = bank()
                mm(p[:, :], [(MGT[:, :], rhs_cum[:, q4 * 4:(q4 + 1) * 4, :])], ["MGT", "RC"], [pk])
                act_op(eb[:, q4 * 4:(q4 + 1) * 4, :], p[:, :].rearrange("p (a t) -> p a t", a=4), AF.Exp,
                       [pk], [ek(q4)])
                rel(pk)

        def A1(n):
            cs = slice(n * 128, (n + 1) * 128)
            p, pk = bank()
            pv = psb(p)
            transposes([(pv[:, f * 128:(f + 1) * 128], xs_fm[:, f, cs]) for f in range(8)], [("RB", "m", 3)], [pk])
            tt("dve", xdt[:].rearrange("p (h q) -> p h q", h=16), pv[:, 0:1024].rearrange("p (h q) -> p h q", h=16),
               bc(dtt[:, n, :], 2, [128, 16, 64]), ALU.mult, [pk, "dtt"], ["xdt"])
            tt("dve", xs_tm[:].rearrange("p (h q) -> p h q", h=16), pv[:, 0:1024].rearrange("p (h q) -> p h q", h=16),
               bc(dsk[:, :], 2, [128, 16, 64]), ALU.mult, [pk, "dsk"], ["xs_tm"])
            rel(pk)
            p, pk = bank()
            pv = psb(p)
            transposes([(pv[:, f * 128:(f + 1) * 128], k_rot[:, f, cs]) for f in range(8)], [("RA", "m", 1)], [pk])
            for hh in range(4):
                act_op(kz_tm[:, hh * 256:(hh + 1) * 256], pv[:, hh * 256:(hh + 1) * 256], AF.Copy, [pk, "zeta16"],
                       [("kz_tm", hh)], scale=zeta16[:, hh:hh + 1])
            rel(pk)
            p, pk = bank()
            pv = psb(p)
            transposes([(pv[:, g * 128:(g + 1) * 128], Bfm[:, g, cs]) for g in range(2)], ["Bfm"], [pk])
            copy("act", B_tm[:], pv[:, 0:256], [pk], ["B_tm"])
            rel(pk)
            tt("pool", qx[:, :, :].rearrange("p (h j) t -> p h j t", h=4),
               q_rot[:, :, cs].rearrange("p (h j) t -> p h j t", h=4),
               bc(xi_b[:, :, :], 2, [128, 4, 2, 128]), ALU.mult, [("RA", "m", 0), "xi_b"], ["qx"])

        def A2(n):
            cs = slice(n * 128, (n + 1) * 128)
            p, pk = bank()
            for hh in range(4):
                mm(p[:, hh * 128:(hh + 1) * 128],
                   [(k_rot[:, 2 * hh + j, cs], q_rot[:, 2 * hh + j, cs]) for j in range(2)],
                   [("RA", "m", 0), ("RA", "m", 1)], [pk])
            tt("dve", scm[:, :, :], p[:, :].rearrange("p (a t) -> p a t", a=4), intraT[:, :, :], ALU.mult,
               [pk, "intraT"], ["scm"])
            rel(pk)

        def A5(n):
            cs = slice(n * 128, (n + 1) * 128)
            p, pk = bank()
            for g in range(2):
                mm(p[:, g * 128:(g + 1) * 128], [(Bfm[:, g, cs], Cfm[:, g, cs])], ["Bfm", "Cfm"], [pk])
            tt("dve", CBm[:, :, :], p[:, 0:256].rearrange("p (g t) -> p g t", g=2), bc(U[:, :], 1, [128, 2, 128]),
               ALU.mult, [pk, "U"], ["CBm"])
            rel(pk)
            p3, pk3 = bank()
            mm(p3[:, 0:16], [(U[:, :], da[:, n, :])], ["U", "da"], [pk3])
            mm(p3[:, 16:32], [(MGT[:, :], da[:, n, :])], ["MGT", "da"], [pk3])
            mm(p3[:, 32:48], [(onesf[:, :], da[:, n, :])], ["onesf", "da"], [pk3])
            act_op(e3[:, :], p3[:, 0:48], AF.Exp, [pk3], ["e3"])
            rel(pk3)

        def A6(n):
            eb, ek = EB[n % 2]
            for g in range(2):
                tt("dve", eb[:, g * 8:(g + 1) * 8, :], eb[:, g * 8:(g + 1) * 8, :],
                   bc(CBm[:, g, :], 1, [128, 8, 128]), ALU.mult,
                   [ek(2 * g), ek(2 * g + 1), "CBm"], [ek(2 * g), ek(2 * g + 1)])
            tt("pool", xw[:].rearrange("p (h q) -> p h q", h=16), xdt[:].rearrange("p (h q) -> p h q", h=16),
               bc(e3[:, 16:32], 2, [128, 16, 64]), ALU.mult, ["xdt", "e3"], ["xw"])

        def Bstep(n):
            cs = slice(n * 128, (n + 1) * 128)
            eb, ek = EB[n % 2]
            yd = [bank(), bank()]
            for hh in range(16):
                p, pk = yd[hh // 8]
                c0 = (hh % 8) * 64
                mm(p[:, c0:c0 + 64], [(eb[:, hh, :], xdt[:, hh * 64:(hh + 1) * 64])],
                   [ek(hh // 4), "xdt"], [pk])
            yo = [bank(), bank()]
            for g in range(2):
                p, pk = yo[g]
                mm(p[:, :], [(Cfm[:, g, cs], Ssb[:, g * 512:(g + 1) * 512])], ["Cfm", "Ssb"], [pk])
            dS = [bank(), bank()]
            for g in range(2):
                p, pk = dS[g]
                mm(p[:, :], [(B_tm[:, g * 128:(g + 1) * 128], xw[:, g * 512:(g + 1) * 512])], ["B_tm", "xw"], [pk])
            for g in range(2):
                p, pk = yo[g]
                tt("dve", t1[:, g * 512:(g + 1) * 512].rearrange("p (h q) -> p h q", h=8),
                   p[:, :].rearrange("p (h q) -> p h q", h=8),
                   bc(e3[:, g * 8:(g + 1) * 8], 2, [128, 8, 64]), ALU.mult, [pk, "e3"], [("t12", 0)])
                rel(pk)
            tt("pool", Ss[:].rearrange("p (h q) -> p h q", h=16), Ss[:].rearrange("p (h q) -> p h q", h=16),
               bc(e3[:, 32:48], 2, [128, 16, 64]), ALU.mult, ["Ss", "e3"], ["Ss"])
            for g in range(2):
                p, pk = dS[g]
                tt("dve", Ss[:, g * 512:(g + 1) * 512], Ss[:, g * 512:(g + 1) * 512], p[:, :], ALU.add,
                   [pk, "Ss"], ["Ss"])
                rel(pk)
            copy("act", Ssb[:], Ss[:], ["Ss"], ["Ssb"])
            tt("pool", t1, t1, xs_tm[:], ALU.add, [("t12", 0), "xs_tm"], [("t12", 0)])
            ob = [bank(), bank()]
            for hh in range(4):
                p, pk = ob[hh // 2]
                c0 = (hh % 2) * 256
                mm(p[:, c0:c0 + 256],
                   [(scm[:, hh, :], vt[:, n, hh * 256:(hh + 1) * 256])] +
                   [(qx[:, 2 * hh + j, :], Srb[:, hh, j * 256:(j + 1) * 256]) for j in range(2)],
                   ["scm", ("RB", "m", 1), "qx", "Srb"], [pk])
            dR = [bank(), bank(), bank(), bank()]
            for hh in range(4):
                p, pk = dR[hh]
                for j in range(2):
                    f = 2 * hh + j
                    mm(p[:, j * 256:(j + 1) * 256],
                       [(kz_tm[:, f * 128:(f + 1) * 128], vt[:, n, hh * 256:(hh + 1) * 256])],
                       ["kz_tm", ("RB", "m", 1)], [pk])
            for hh in range(4):
                p, pk = dR[hh]
                cd = (1.0 - 2.0 ** (-5.0 - hh)) ** 128
                stt(Sr[:, hh, :], Sr[:, hh, :], cd, p[:, :], ALU.mult, ALU.add, [pk, "Sr"], [("Sr", hh)])
                rel(pk)
            copy("act", Srb[:, :, :], Sr[:, :, :], ["Sr"], ["Srb"])
            held[n] = (yd, ob)

        def C1(m):
            yd, ob = held[m]
            for g in range(2):
                p, pk = yd[g]
                tt("dve", t1[:, g * 512:(g + 1) * 512], t1[:, g * 512:(g + 1) * 512], p[:, :], ALU.add,
                   [pk, ("t12", 0)], [("t12", 0)])
                rel(pk)

        def C2b(m):
            tt("dve", ysc[:], t1, zs[:, m, :], ALU.mult, [("t12", 0), ("RB", "m", 0)], ["hs"])

        def C3a(m):
            st, sk = small()
            act_op(yr[:], ysc[:], AF.Square, ["hs"], ["yr", sk], accum_out=st[:, 0:1])
            ts("pool", st[:, 1:2], st[:, 0:1], 1.0 / D, EPS, ALU.mult, ALU.add, [sk], [sk])
            tt("pool", rs_ssd[:, m:m + 1], st[:, 1:2], neghalf[:, 0:1], ALU.pow, [sk, "neghalf"], [("rs_ssd", m)])

        def C3b(m):
            cs = slice(m * 128, (m + 1) * 128)
            p, pk = bank()
            pv = psb(p)
            transposes([(pv[:, f * 128:(f + 1) * 128], ysc[:, f * 128:(f + 1) * 128]) for f in range(8)], ["hs"], [pk])
            tt("dve", yT[:, 0:8, cs], pv[:, 0:1024].rearrange("p (a t) -> p a t", a=8),
               bc(gssd[:, 0:8], 2, [128, 8, 128]), ALU.mult, [pk, "gssd"], [("RA", "m", 2)])
            rel(pk)

        cst2 = {}

        def C4a(m):
            yd, ob = held[m]
            st, sk = small()
            for hh in range(4):
                p, pk = ob[hh // 2]
                c0 = (hh % 2) * 256
                act_op(yr[:, hh * 256:(hh + 1) * 256], p[:, c0:c0 + 256], AF.Square, [pk], ["yr", sk],
                       accum_out=st[:, hh:hh + 1])
            st2, sk2 = small()
            ts("pool", st2[:, :], st[:, :], 1.0 / 256, EPS, ALU.mult, ALU.add, [sk], [sk2])
            tt("pool", st2[:, :], st2[:, :], neghalf[:, :], ALU.pow, [sk2, "neghalf"], [sk2])
            cst2[m] = (st2, sk2)

        def C4b(m):
            cs = slice(m * 128, (m + 1) * 128)
            yd, ob = held[m]
            st2, sk2 = cst2[m]
            for hh in range(4):
                p, pk = ob[hh // 2]
                c0 = (hh % 2) * 256
                stt(yr[:, hh * 256:(hh + 1) * 256], p[:, c0:c0 + 256], st2[:, hh:hh + 1],
                    gs[:, m, hh * 256:(hh + 1) * 256], ALU.mult, ALU.mult, [pk, sk2, ("RB", "m", 2)], ["yr"])
            rel(ob[0][1], ob[1][1])
            p, pk = bank()
            pv = psb(p)
            transposes([(pv[:, f * 128:(f + 1) * 128], yr[:, f * 128:(f + 1) * 128]) for f in range(8)], ["yr"], [pk])
            copy("act", yT[:, 8:16, cs], pv[:, 0:1024].rearrange("p (a t) -> p a t", a=8), [pk], [("RA", "m", 3)])
            rel(pk)

        def AC(n, m):
            seq = [("C", C4a), ("A", A1), ("C", C1), ("A", A5), ("C", C2b), ("A", A2), ("C", C4b), ("A", A6),
                   ("C", C3a), ("C", C3b)]
            for kind, fn in seq:
                if kind == "A" and n is not None:
                    fn(n)
                if kind == "C" and m is not None:
                    fn(m)

        rhs(0)
        A3(0)
        AC(0, None)
        for n in range(CH):
            if n + 1 < CH:
                rhs(n + 1)
            Bstep(n)
            if n + 1 < CH:
                A3(n + 1)
                AC(n + 1, n)
            else:
                AC(None, n)

        if nxt is not None:
            rope_tables(*nxt)

        for cg in range(2):
            for kh in range(2):
                w, wk = wload("w_out", kh * 1024, 8, cg * 512, 512)
                for tc in range(CH):
                    p, pk = bank()
                    mm(p[:, :], [(yT[:, kh * 8 + kt, tc * 128:(tc + 1) * 128], w[:, kt, :]) for kt in range(8)],
                       [wk, ("RA", "m", 2 + kh)], [pk])
                    hsl = h[:, tc, cg * 512:(cg + 1) * 512]
                    if kh == 0:
                        stt(hsl, p[:, :], rs_ssd[:, tc:tc + 1], hsl, ALU.mult, ALU.add,
                            [pk, ("h", tc), ("rs_ssd", tc)], [("h", tc)])
                    else:
                        tt("dve", hsl, hsl, p[:, :], ALU.add, [pk, ("h", tc)], [("h", tc)])
                    rel(pk)

        norms_to_fm(gxat, "gxat")
        for half in range(2):
            w, wk = wload("w_xq", 0, 8, half * 512, 512)
            for j in range(4):
                p, pk = bank()
                mm(p[:, :], [(w[:, kt, j * 128:(j + 1) * 128], hnT[:, kt, :]) for kt in range(8)], [wk, "hnT"], [pk])
                copy("act", QT[:, half * 4 + j, :], p[:, :], [pk], [("RB", "x", 0)])
                rel(pk)
        for hh in range(4):
            for mc in range(2):
                p, pk = bank()
                mm(p[:, :], [(KT[:, 2 * hh + j, mc * 128:(mc + 1) * 128], QT[:, 2 * hh + j, :]) for j in range(2)],
                   ["KT", ("RB", "x", 0)], [pk])
                act_op(PT[:, hh * 2 + mc, :], p[:, :], AF.Exp, [pk], [("RB", "x", 1)], scale=1.0 / 16.0)
                rel(pk)
        for hh in range(4):
            p, pk = bank()
            mm(p[:, :], [(onesb[:, :], PT[:, hh * 2 + mc, :]) for mc in range(2)], ["onesb", ("RB", "x", 1)], [pk])
            P.op("dve", (lambda e, p=p, hh=hh: e.reciprocal(out=rinv[:, hh, :], in_=p[:, :])), reads=[pk],
                 writes=[("RB", "x", 2)])
            rel(pk)
            for j in range(2):
                p, pk = bank()
                f = 2 * hh + j
                mm(p[:, :], [(Vm[:, mc, f * 128:(f + 1) * 128], PT[:, hh * 2 + mc, :]) for mc in range(2)],
                   ["Vm", ("RB", "x", 1)], [pk])
                tt("dve", oTn[:, f, :], p[:, :], rinv[:, hh, :], ALU.mult, [pk, ("RB", "x", 2)], [("RB", "x", 3)])
                rel(pk)
        for cg in range(2):
            w, wk = wload("w_xo", 0, 8, cg * 512, 512)
            for tc in range(CH):
                p, pk = bank()
                mm(p[:, :], [(oTn[:, kt, tc * 128:(tc + 1) * 128], w[:, kt, :]) for kt in range(8)],
                   [wk, ("RB", "x", 3)], [pk])
                tt("dve", h[:, tc, cg * 512:(cg + 1) * 512], h[:, tc, cg * 512:(cg + 1) * 512], p[:, :], ALU.add,
                   [pk, ("h", tc)], [("h", tc)])
                rel(pk)

        norms_to_fm(gffn, "gffn")
        for ci in range(11):
            if ci == 4 and nxt is not None:
                nb_, nblk_ = nxt
                for tc in range(CH):
                    dma_in(f"xld{tc}", xpre[:, tc, :],
                           x_d[nb_, nblk_ * T + tc * 128:nblk_ * T + (tc + 1) * 128, :], writes=[("RB", "p", tc)])
            w, wk = wload("w_gu", 0, 8, ci * 512, 512)
            for jj in range(2):
                j = ci * 2 + jj
                pg, pgk = bank()
                pu, puk = bank()
                mm(pg[:, :], [(w[:, kt, jj * 256:jj * 256 + 128], hnT[:, kt, :]) for kt in range(8)], [wk, "hnT"], [pgk])
                mm(pu[:, :], [(w[:, kt, jj * 256 + 128:jj * 256 + 256], hnT[:, kt, :]) for kt in range(8)],
                   [wk, "hnT"], [puk])
                act_op(sg, pg[:, :], AF.Silu, [pgk], ["hs"])
                tt("dve", act[:, j, :], sg, pu[:, :], ALU.mult, [puk, "hs"], [("RA", "f", j)])
                rel(pgk, puk)
        if nxt is not None:
            norms_to_fm(gmix, "gmix", src=xpre)
        KSPL = [(0, 8), (8, 8), (16, 6)]
        for cg in range(2):
            banks4 = [bank() for _ in range(CH)]
            for ki, (k0, kn) in enumerate(KSPL):
                w, wk = wload("w_down", k0 * 128, kn, cg * 512, 512)
                for tc in range(CH):
                    p, pk = banks4[tc]

                    def fn(e, p=p, w=w, k0=k0, kn=kn, tc=tc, ki=ki):
                        ins = None
                        for kt in range(kn):
                            ins = e.matmul(p[:, :], lhsT=act[:, k0 + kt, tc * 128:(tc + 1) * 128], rhs=w[:, kt, :],
                                           start=(ki == 0 and kt == 0), stop=(ki == 2 and kt == kn - 1))
                        return ins
                    P.op("pe", fn, reads=[wk, ("RA", "f", None)], writes=[pk])
            for tc in range(CH):
                p, pk = banks4[tc]
                tt("dve", h[:, tc, cg * 512:(cg + 1) * 512], h[:, tc, cg * 512:(cg + 1) * 512], p[:, :], ALU.add,
                   [pk, ("h", tc)], [("h", tc)])
                rel(pk)

        sts = []
        for tc in range(CH):
            st, sk = small()
            act_op(hs[:], h[:, tc, :], AF.Square, [("h", tc)], ["hs", sk], accum_out=st[:, 0:1])
            sts.append((st, sk))
        for tc in range(CH):
            st, sk = sts[tc]
            ts("pool", st[:, 1:2], st[:, 0:1], 1.0 / D, EPS, ALU.mult, ALU.add, [sk], [sk])
            tt("pool", st[:, 2:3], st[:, 1:2], neghalf[:, 0:1], ALU.pow, [sk, "neghalf"], [sk])
        for tc in range(CH):
            st, sk = sts[tc]
            par = tc % 2
            stt(t12[:, par, :], h[:, tc, :], st[:, 2:3], gfin[:, :], ALU.mult, ALU.mult,
                [("h", tc), sk, "gfin"], [("t12", par)])
            P.op("pool", (lambda e, par=par, tc=tc: e.dma_start(
                out=out_d[b, t0 + tc * 128:t0 + (tc + 1) * 128, :], in_=t12[:, par, :])),
                reads=[("t12", par)], writes=[("outdram", (b * 100 + blk) * 4 + tc)], dma=f"ost{par}")

    P.unordered.add("consts")
    prologue()
    order = [(b, blk) for b in range(nseq) for blk in range(nblk)]
    for i, (b, blk) in enumerate(order):
        if blk == 0:
            seq_setup(b)
        if i == 0:
            rope_tables(b, blk)
        block(b, blk, order[i + 1] if i + 1 < len(order) else None, i > 0)
    P.final.append(("pool", "ost0"))
    P.final.append(("pool", "ost1"))
    P.emit(nc, es)
    es.close()
    return nc, P


def _consts():
    i = np.arange(128)
    ident = np.eye(128, dtype=np.float32)
    Um = (i[:, None] <= i[None, :]).astype(np.float32)
    MGT = (i[:, None] > i[None, :]).astype(np.float32)
    gam = 1.0 - 2.0 ** (-5.0 - np.arange(4, dtype=np.float64))
    rel = (i[None, :] - i[:, None]).astype(np.float64)
    intraT = np.zeros((128, 4, 128), np.float32)
    xi = np.zeros((128, 4, 128), np.float32)
    zeta = np.zeros((128, 4), np.float32)
    for hh in range(4):
        intraT[:, hh, :] = np.where(rel >= 0, gam[hh] ** np.maximum(rel, 0), 0.0) / 16.0
        xi[:, hh, :] = (gam[hh] ** (i + 1.0))[None, :]
        zeta[:, hh] = gam[hh] ** (127.0 - i) / 16.0
    invf = (1.0 / (10000.0 ** np.linspace(0.0, 1.0, 128, dtype=np.float32))).astype(np.float32)[:, None]
    return dict(c_ident=ident, c_U=Um, c_MGT=MGT, c_intraT=intraT, c_xi=xi, c_zeta=zeta, c_invf=invf)


def prep_shared(inp):
    f = lambda a: np.ascontiguousarray(np.asarray(a, dtype=np.float32))
    w_in = f(inp["w_in"])[0]
    z, xbc, dtw = w_in[:, 0:1024], w_in[:, 1024:2560], w_in[:, 2560:2576]
    q, k, v, g = (w_in[:, 2576 + i * 1024:2576 + (i + 1) * 1024] for i in range(4))
    perm = np.concatenate([np.concatenate([np.arange(0, 256, 2), np.arange(1, 256, 2)]) + hh * 256 for hh in range(4)])
    wg, wu = f(inp["w_gate"])[0], f(inp["w_up"])[0]
    w_gu = np.stack([wg.reshape(1024, 22, 128), wu.reshape(1024, 22, 128)], axis=2).reshape(1024, 5632)
    fm = lambda a: np.ascontiguousarray(f(a).reshape(8, 128).T)
    rep = lambda a: np.ascontiguousarray(np.tile(f(a).reshape(1, -1), (128, 1)))
    cw = f(inp["conv_w"])[0]
    sh = dict(
        w_fm=np.ascontiguousarray(np.concatenate([xbc, q[:, perm], k[:, perm]], axis=1)),
        w_tm=np.ascontiguousarray(np.concatenate([z, v, g], axis=1)),
        w_dt=np.ascontiguousarray(dtw),
        w_out=f(inp["w_out"])[0], w_xq=f(inp["w_xq"])[0], w_xk=f(inp["w_xk"])[0], w_xv=f(inp["w_xv"])[0],
        w_xo=f(inp["w_xo"])[0], w_gu=np.ascontiguousarray(w_gu), w_down=f(inp["w_down"])[0],
        g_mix=fm(inp["norm_mix_g"]), g_xat=fm(inp["norm_xattn_g"]), g_ffn=fm(inp["norm_ffn_g"]),
        g_ssd=fm(inp["ssd_norm_g"]), g_fin=rep(inp["norm_final_g"]),
        convw=np.ascontiguousarray(cw.T.reshape(12, 128, 4).transpose(1, 0, 2)),
        convb=np.ascontiguousarray(f(inp["conv_b"])[0].reshape(12, 128).T),
        dtb=rep(inp["dt_bias"]), alog=rep(inp["a_log"]), dsk=rep(inp["d_skip"]),
    )
    sh.update(_consts())
    return sh


def kernel(**inputs):
    sh = prep_shared(inputs)
    x = np.asarray(inputs["x"], dtype=np.float32)
    mem = np.asarray(inputs["mem"], dtype=np.float32)
    pos = np.asarray(inputs["positions"], dtype=np.int32)
    nseq = BATCH // NCORES
    nc, _ = build_program(nseq=nseq, nblk=SEQ // T)
    in_maps = []
    for c in range(NCORES):
        m = dict(sh)
        m["x"] = np.ascontiguousarray(x[c * nseq:(c + 1) * nseq])
        m["mem"] = np.ascontiguousarray(mem[c * nseq:(c + 1) * nseq])
        m["pos"] = np.ascontiguousarray(pos[c * nseq:(c + 1) * nseq])
        in_maps.append(m)
    res = run_bass_kernel_spmd(nc, in_maps, core_ids=list(range(NCORES)))
    return np.concatenate([np.asarray(r["out"]) for r in res.results], axis=0).astype(np.float32)
```
